# Optimizing a Trainium2 kernel written in Bass

```python
import jax, jax.numpy as jnp
from jax import lax
import numpy as np

D_MODEL = 1024
BATCH = 8
SEQ = 4096
DEPTH = 4

D_MIX = D_MODEL
ATT_HEAD_DIM = 64
ATT_WIDTH = D_MIX // 4
ATT_HEADS = ATT_WIDTH // ATT_HEAD_DIM
DILATED_BRANCHES = ((128, 1), (512, 4), (2048, 16))
ATT_BLOCK = 128
POOL_WINDOWS = (2, 4, 8, 16)
POOL_WIDTH = D_MIX // 4
POOL_GROUP_DIM = POOL_WIDTH // len(POOL_WINDOWS)
MLSTM_WIDTH = D_MIX - ATT_WIDTH - POOL_WIDTH
MLSTM_HEADS = 4
MLSTM_HEAD_DIM = MLSTM_WIDTH // MLSTM_HEADS
MLSTM_CONV = 4
MLSTM_CHUNK = 64
IN_WIDTH = 3 * ATT_WIDTH + POOL_WIDTH + 2 * MLSTM_WIDTH + 2 * MLSTM_HEADS
D_FF = ((8 * D_MODEL // 3 + 255) // 256) * 256
MACARON_WEIGHT = 0.5
EPS = 1e-6
NEG = -1e30

kernel_name = "hybrid_dilated_pool_mlstm_macaron"


def rmsnorm(x, w):
    xf = x.astype(jnp.float32)
    y = xf * lax.rsqrt(jnp.mean(xf * xf, axis=-1, keepdims=True) + EPS)
    return (y * w.astype(jnp.float32)).astype(x.dtype)


def swiglu(u, w_up, w_down):
    g, v = jnp.split(u @ w_up, 2, axis=-1)
    return (jax.nn.silu(g) * v) @ w_down


def dilated_branch(q, k, v, window, dilation):
    B, H, S, Dh = q.shape
    Q = ATT_BLOCK
    span = window // dilation
    L = S // dilation
    nb = -(-L // Q)
    Lp = nb * Q

    def strided(t):
        t = t.reshape(B, H, L, dilation, Dh).transpose(0, 1, 3, 2, 4)
        return jnp.pad(t, ((0, 0), (0, 0), (0, 0), (0, Lp - L), (0, 0)))

    def key_blocks(t):
        t = jnp.pad(t, ((0, 0), (0, 0), (0, 0), (Q, 0), (0, 0))).reshape(B, H, dilation, nb + 1, Q, Dh)
        return jnp.concatenate([t[:, :, :, :-1], t[:, :, :, 1:]], axis=4)

    qb = strided(q).reshape(B, H, dilation, nb, Q, Dh)
    kb = key_blocks(strided(k))
    vb = key_blocks(strided(v))
    qi = jnp.arange(nb)[:, None, None] * Q + jnp.arange(Q)[None, :, None]
    ki = jnp.arange(nb)[:, None, None] * Q - Q + jnp.arange(2 * Q)[None, None, :]
    dist = qi - ki
    mask = (dist >= 0) & (dist <= span) & (ki >= 0)
    s = jnp.einsum('bhrnqd,bhrnkd->bhrnqk', qb, kb) * (Dh ** -0.5)
    s = jnp.where(mask, s, NEG)
    m = jnp.max(s, axis=-1)
    p = jnp.exp(s - m[..., None])
    l = jnp.sum(p, axis=-1)
    o = jnp.einsum('bhrnqk,bhrnkd->bhrnqd', p, vb) / l[..., None]

    def unstrided(t):
        tail = t.shape[5:]
        t = t.reshape((B, H, dilation, Lp) + tail)[:, :, :, :L]
        t = jnp.moveaxis(t, 2, 3)
        return t.reshape((B, H, S) + tail)

    return unstrided(o), unstrided(m), unstrided(l)


def dilated_attention(q, k, v):
    q, k, v = (t.astype(jnp.float32) for t in (q, k, v))
    outs = [dilated_branch(q, k, v, w, d) for (w, d) in DILATED_BRANCHES]
    o_all = jnp.stack([o for o, _, _ in outs])
    m_all = jnp.stack([m for _, m, _ in outs])
    l_all = jnp.stack([l for _, _, l in outs])
    wts = l_all * jnp.exp(m_all - jnp.max(m_all, axis=0, keepdims=True))
    wts = wts / jnp.sum(wts, axis=0, keepdims=True)
    return jnp.sum(wts[..., None] * o_all, axis=0)


def pool_mixer(p, pool_w, pool_scale):
    B, S, C = p.shape
    G = POOL_GROUP_DIM
    pf = p.astype(jnp.float32)
    cs = jnp.cumsum(pf, axis=1)
    count = jnp.arange(1, S + 1, dtype=jnp.float32)[None, :, None]
    outs = []
    for g, w in enumerate(POOL_WINDOWS):
        c_g = cs[:, :, g * G:(g + 1) * G]
        lag = jnp.pad(c_g[:, :S - w], ((0, 0), (w, 0), (0, 0)))
        outs.append((c_g - lag) / jnp.minimum(count, w) - pf[:, :, g * G:(g + 1) * G])
    d = jnp.stack(outs, axis=2)
    y = jnp.einsum('bsgc,gce->bsge', d, pool_w.astype(jnp.float32)).reshape(B, S, C)
    return (y * pool_scale).astype(p.dtype)


def causal_conv(x, w, b):
    K = w.shape[0]
    S = x.shape[1]
    xp = jnp.pad(x, ((0, 0), (K - 1, 0), (0, 0)))
    y = xp[:, 0:S] * w[0]
    for t in range(1, K):
        y = y + xp[:, t:t + S] * w[t]
    return y + b


def mlstm_chunkwise(q, k, v, i_pre, f_pre):
    B, H, S, dh = q.shape
    L = MLSTM_CHUNK
    NC = S // L
    qc = q.astype(jnp.float32).reshape(B, H, NC, L, dh) * (dh ** -0.5)
    kc = k.astype(jnp.float32).reshape(B, H, NC, L, dh)
    vc = v.astype(jnp.float32).reshape(B, H, NC, L, dh)
    ic = i_pre.astype(jnp.float32).reshape(B, H, NC, L)
    logf = jax.nn.log_sigmoid(f_pre.astype(jnp.float32)).reshape(B, H, NC, L)
    b = jnp.cumsum(logf, axis=-1)
    b_last = b[..., -1]
    causal = jnp.tril(jnp.ones((L, L), dtype=bool))
    logD = jnp.where(causal, b[..., :, None] - b[..., None, :] + ic[..., None, :], NEG)
    a = b_last[..., None] - b + ic
    a_max = jnp.max(a, axis=-1)
    wa = jnp.exp(a - a_max[..., None])
    dC = jnp.einsum('bhcl,bhcld,bhcle->bhcde', wa, kc, vc)
    dn = jnp.einsum('bhcl,bhcld->bhcd', wa, kc)

    def step(carry, xs):
        C, n, m = carry
        bl, am, dC_c, dn_c = xs
        m_new = jnp.maximum(bl + m, am)
        decay = jnp.exp(bl + m - m_new)
        inj = jnp.exp(am - m_new)
        C_new = decay[..., None, None] * C + inj[..., None, None] * dC_c
        n_new = decay[..., None] * n + inj[..., None] * dn_c
        return (C_new, n_new, m_new), (C, n, m)

    init = (jnp.zeros((B, H, dh, dh), jnp.float32), jnp.zeros((B, H, dh), jnp.float32),
            jnp.zeros((B, H), jnp.float32))
    xs = (jnp.moveaxis(b_last, 2, 0), jnp.moveaxis(a_max, 2, 0), jnp.moveaxis(dC, 2, 0), jnp.moveaxis(dn, 2, 0))
    _, (C_prev, n_prev, m_prev) = lax.scan(step, init, xs)
    C_prev = jnp.moveaxis(C_prev, 0, 2)
    n_prev = jnp.moveaxis(n_prev, 0, 2)
    m_prev = jnp.moveaxis(m_prev, 0, 2)
    inter_log = b + m_prev[..., None]
    m_t = jnp.maximum(inter_log, jnp.max(logD, axis=-1))
    inter_w = jnp.exp(inter_log - m_t)
    sw = jnp.einsum('bhcld,bhcsd->bhcls', qc, kc) * jnp.exp(logD - m_t[..., None])
    num = inter_w[..., None] * jnp.einsum('bhcld,bhcde->bhcle', qc, C_prev) + jnp.einsum('bhcls,bhcse->bhcle', sw, vc)
    den = inter_w * jnp.einsum('bhcld,bhcd->bhcl', qc, n_prev) + jnp.sum(sw, axis=-1)
    h = num / jnp.maximum(jnp.abs(den), jnp.exp(-m_t))[..., None]
    return h.reshape(B, H, S, dh)


def hybrid_mixer(u, w_in, w_out, pool_w, pool_scale, conv_w, conv_b, qkv_w, gate_b, norm_w, skip):
    B, S, _ = u.shape
    widths = [ATT_WIDTH, ATT_WIDTH, ATT_WIDTH, POOL_WIDTH, MLSTM_WIDTH, MLSTM_WIDTH, MLSTM_HEADS, MLSTM_HEADS]
    idx = []
    acc = 0
    for wd in widths[:-1]:
        acc += wd
        idx.append(acc)
    aq, ak, av, p_in, xm, og, ig, fg = jnp.split(u @ w_in, idx, axis=-1)

    def heads(t, n):
        return t.reshape(B, S, n, -1).transpose(0, 2, 1, 3)

    att = dilated_attention(heads(aq, ATT_HEADS), heads(ak, ATT_HEADS), heads(av, ATT_HEADS))
    att = att.transpose(0, 2, 1, 3).reshape(B, S, ATT_WIDTH).astype(u.dtype)
    pool = pool_mixer(p_in, pool_w, pool_scale)
    xc = jax.nn.silu(causal_conv(xm, conv_w, conv_b))
    xch = xc.reshape(B, S, MLSTM_HEADS, MLSTM_HEAD_DIM)
    xmh = xm.reshape(B, S, MLSTM_HEADS, MLSTM_HEAD_DIM)
    q = jnp.einsum('bshd,hde->bhse', xch, qkv_w[0])
    k = jnp.einsum('bshd,hde->bhse', xch, qkv_w[1])
    v = jnp.einsum('bshd,hde->bhse', xmh, qkv_w[2])
    i_pre = (ig + gate_b[0]).transpose(0, 2, 1)
    f_pre = (fg + gate_b[1]).transpose(0, 2, 1)
    h = mlstm_chunkwise(q, k, v, i_pre, f_pre)
    h = h * jax.nn.sigmoid(heads(og, MLSTM_HEADS).astype(jnp.float32))
    h = rmsnorm(h, norm_w.reshape(MLSTM_HEADS, 1, MLSTM_HEAD_DIM))
    h = h.transpose(0, 2, 1, 3).reshape(B, S, MLSTM_WIDTH).astype(u.dtype) + skip * xc
    mixed = jnp.concatenate([att, pool, h], axis=-1)
    return mixed @ w_out


def sublayer(h, mod_j, pre_w, post_w, fn, weight):
    shift, scale, gate = mod_j[:, 0], mod_j[:, 1], mod_j[:, 2]
    u = rmsnorm(h, pre_w) * (1 + scale) + shift
    return h + weight * gate * rmsnorm(fn(u), post_w)


def setup_inputs(seed: int = 0) -> dict:
    key = jax.random.key(seed)
    ks = jax.random.split(key, 20)

    def nrm(k, shape, s):
        return jax.random.normal(k, shape, jnp.float32) * s

    x = nrm(ks[0], (BATCH, SEQ, D_MODEL), 1.0)
    c = nrm(ks[1], (BATCH, D_MODEL), 1.0)
    ada_w = nrm(ks[2], (DEPTH, D_MODEL, 9 * D_MODEL), D_MODEL ** -0.5)
    ada_b = nrm(ks[3], (DEPTH, 9 * D_MODEL), 0.02)
    pre_norm_w = 1.0 + nrm(ks[4], (DEPTH, 3, D_MODEL), 0.05)
    post_norm_w = 1.0 + nrm(ks[5], (DEPTH, 3, D_MODEL), 0.05)
    ffn_up = nrm(ks[6], (DEPTH, 2, D_MODEL, 2 * D_FF), D_MODEL ** -0.5)
    ffn_down = nrm(ks[7], (DEPTH, 2, D_FF, D_MODEL), D_FF ** -0.5)
    mix_in_w = nrm(ks[8], (DEPTH, D_MODEL, IN_WIDTH), D_MODEL ** -0.5)
    mix_out_w = nrm(ks[9], (DEPTH, D_MIX, D_MODEL), D_MIX ** -0.5)
    pool_w = nrm(ks[10], (DEPTH, len(POOL_WINDOWS), POOL_GROUP_DIM, POOL_GROUP_DIM), POOL_GROUP_DIM ** -0.5)
    pool_scale = 1.0 + nrm(ks[11], (DEPTH, POOL_WIDTH), 0.1)
    mlstm_conv_w = nrm(ks[12], (DEPTH, MLSTM_CONV, MLSTM_WIDTH), MLSTM_CONV ** -0.5)
    mlstm_conv_b = nrm(ks[13], (DEPTH, MLSTM_WIDTH), 0.02)
    mlstm_qkv_w = nrm(ks[14], (DEPTH, 3, MLSTM_HEADS, MLSTM_HEAD_DIM, MLSTM_HEAD_DIM), MLSTM_HEAD_DIM ** -0.5)
    i_b = nrm(ks[15], (DEPTH, MLSTM_HEADS), 0.1)
    f_b = jnp.linspace(3.0, 6.0, MLSTM_HEADS, dtype=jnp.float32)[None, :] + nrm(ks[16], (DEPTH, MLSTM_HEADS), 0.1)
    mlstm_gate_b = jnp.stack([i_b, f_b], axis=1)
    mlstm_norm_w = 1.0 + nrm(ks[17], (DEPTH, MLSTM_WIDTH), 0.05)
    mlstm_skip = 1.0 + nrm(ks[18], (DEPTH, MLSTM_WIDTH), 0.1)
    return {"x": x, "c": c, "ada_w": ada_w, "ada_b": ada_b, "pre_norm_w": pre_norm_w,
            "post_norm_w": post_norm_w, "ffn_up": ffn_up, "ffn_down": ffn_down, "mix_in_w": mix_in_w,
            "mix_out_w": mix_out_w, "pool_w": pool_w, "pool_scale": pool_scale,
            "mlstm_conv_w": mlstm_conv_w, "mlstm_conv_b": mlstm_conv_b, "mlstm_qkv_w": mlstm_qkv_w,
            "mlstm_gate_b": mlstm_gate_b, "mlstm_norm_w": mlstm_norm_w, "mlstm_skip": mlstm_skip}


def reference(x, c, ada_w, ada_b, pre_norm_w, post_norm_w, ffn_up, ffn_down, mix_in_w, mix_out_w,
              pool_w, pool_scale, mlstm_conv_w, mlstm_conv_b, mlstm_qkv_w, mlstm_gate_b, mlstm_norm_w,
              mlstm_skip):
    B = x.shape[0]
    c_act = jax.nn.silu(c)
    h = x
    for l in range(DEPTH):
        mod = (c_act @ ada_w[l] + ada_b[l]).reshape(B, 3, 3, 1, D_MODEL)
        h = sublayer(h, mod[:, 0], pre_norm_w[l, 0], post_norm_w[l, 0],
                     lambda u: swiglu(u, ffn_up[l, 0], ffn_down[l, 0]), MACARON_WEIGHT)
        h = sublayer(h, mod[:, 1], pre_norm_w[l, 1], post_norm_w[l, 1],
                     lambda u: hybrid_mixer(u, mix_in_w[l], mix_out_w[l], pool_w[l], pool_scale[l],
                                            mlstm_conv_w[l], mlstm_conv_b[l], mlstm_qkv_w[l],
                                            mlstm_gate_b[l], mlstm_norm_w[l], mlstm_skip[l]), 1.0)
        h = sublayer(h, mod[:, 2], pre_norm_w[l, 2], post_norm_w[l, 2],
                     lambda u: swiglu(u, ffn_up[l, 1], ffn_down[l, 1]), MACARON_WEIGHT)
    return h
```

```python
import contextlib
import numpy as np
import concourse.bass as bass
import concourse.mybir as mybir
from concourse.bass_utils import run_bass_kernel_spmd

F32 = mybir.dt.float32
BF16 = mybir.dt.bfloat16
AF = mybir.ActivationFunctionType
ALU = mybir.AluOpType

S = 4096
D = 1024
DFF = 2816
NL = 4
INW = 2056
EPS = 1e-6

CT, ADAB, PREW, POSTW, PSC, CW, CB, NW, SK, NV = 0, 8, 296, 392, 488, 496, 560, 576, 592, 608


class Buf:
    __slots__ = ("name", "last_w", "readers", "excl")

    def __init__(self, name, excl=False):
        self.name = name
        self.last_w = None
        self.readers = []
        self.excl = excl


class Op:
    __slots__ = ("idx", "eng", "fn", "dma", "deps", "signal", "count", "sem")


class _Rec:
    def __init__(self):
        self.call = None

    def __getattr__(self, name):
        def f(*a, **kw):
            assert self.call is None
            self.call = (name, a, kw)
            return None
        return f


class Prog:
    EPOCH = 30000
    NDMASEM = 24
    NSWSEM = 12

    def __init__(self, nc):
        self.nc = nc
        self.ops = []
        self.q = {"pe": [], "act": [], "dve": [], "pool": [], "sp": []}
        self._bar_idx = 0

    def op(self, eng, fn, reads=(), writes=(), dma=False):
        o = Op()
        o.idx = len(self.ops)
        o.eng = eng
        rec = _Rec()
        fn(rec)
        name, a, kw = rec.call
        o.fn = (lambda e, name=name, a=a, kw=kw: getattr(e, name)(*a, **kw))
        o.dma = dma
        o.signal = False
        o.count = None
        o.sem = None
        deps = set()
        if any(r.excl for r in reads):
            writes = list(writes) + [r for r in reads if r.excl and r not in writes]
            reads = [r for r in reads if not r.excl]
        for r in reads:
            if r.last_w is not None:
                deps.add(r.last_w)
        for w in writes:
            if w.last_w is not None:
                deps.add(w.last_w)
            for rr in w.readers:
                deps.add(rr)
        for r in reads:
            r.readers.append(o.idx)
        for w in writes:
            w.last_w = o.idx
            w.readers = []
        deps.discard(o.idx)
        o.deps = deps
        self.ops.append(o)
        self.q[eng].append(o)
        return o

    def pe(self, fn, reads=(), writes=()):
        return self.op("pe", fn, reads, writes)

    def act(self, fn, reads=(), writes=()):
        return self.op("act", fn, reads, writes)

    def dve(self, fn, reads=(), writes=()):
        return self.op("dve", fn, reads, writes)

    def pool(self, fn, reads=(), writes=()):
        return self.op("pool", fn, reads, writes)

    def dma(self, q, out, in_, reads=(), writes=(), **kw):
        return self.op(q, lambda e: e.dma_start(out=out, in_=in_, **kw), reads, writes, dma=True)

    def barrier(self):
        last = {}
        dmas = []
        for o in self.ops:
            if o.dma:
                if o.idx >= self._bar_idx:
                    dmas.append(o.idx)
            else:
                last[o.eng] = o.idx
        deps = set(last.values()) | set(dmas)
        self._bar_idx = len(self.ops)
        for eng in ["pe", "act", "dve", "pool", "sp"]:
            o = self.op(eng, lambda e: e.nop(), (), ())
            o.deps = set(deps)

    def emit(self):
        nc = self.nc
        ops = self.ops
        for cls, nds, base in (("sw", self.NSWSEM, 0), ("hw", self.NDMASEM, self.NSWSEM)):
            dma_ops = [o for o in ops if o.dma and ((o.eng == "pool") == (cls == "sw"))]
            if not dma_ops:
                continue
            nds = min(nds, len(dma_ops))
            last_on_sem = [None] * nds
            cnt_on_sem = [0] * nds
            for i, o in enumerate(dma_ops):
                s = i % nds
                if last_on_sem[s] is not None:
                    o.deps.add(last_on_sem[s])
                last_on_sem[s] = o.idx
                cnt_on_sem[s] += 16
                o.sem = ("dma", base + s)
                o.count = cnt_on_sem[s]
                o.signal = True
        for o in ops:
            nd = set()
            for d in o.deps:
                p = ops[d]
                if (not p.dma) and p.eng == "pe" and o.eng == "pe" and not o.dma:
                    continue
                nd.add(d)
            o.deps = nd
            for d in nd:
                ops[d].signal = True
        for eng, lst in self.q.items():
            ep = 0
            c = 0
            for o in lst:
                if o.dma or not o.signal:
                    continue
                if c >= self.EPOCH:
                    ep += 1
                    c = 0
                c += 1
                o.sem = (eng, ep)
                o.count = c
        semkeys = sorted(set(o.sem for o in ops if o.sem is not None))
        self.nsem = len(semkeys)
        sems = {}
        stack = contextlib.ExitStack()
        for k in semkeys:
            sems[k] = stack.enter_context(nc.semaphore("s_%s_%d" % k))
        engobj = {"pe": "tensor", "act": "scalar", "dve": "vector", "pool": "gpsimd", "sp": "sync"}
        self.nwaits = 0
        self.trace = {}
        with stack:
            with nc.Block() as block:
                def make(eng):
                    lst = self.q[eng]

                    def body(e):
                        waited = {}
                        maxep = {}
                        tr = self.trace.setdefault(eng, [])
                        for o in lst:
                            wl = []
                            need = {}
                            for d in o.deps:
                                p = ops[d]
                                k = p.sem
                                if need.get(k, 0) < p.count:
                                    need[k] = p.count
                            for k in sorted(need):
                                c = need[k]
                                if waited.get(k, 0) >= c:
                                    continue
                                if k[0] != "dma" and maxep.get(k[0], -1) > k[1]:
                                    continue
                                e.wait_ge(sems[k], c)
                                wl.append((k, c))
                                self.nwaits += 1
                                waited[k] = c
                                if k[0] != "dma":
                                    maxep[k[0]] = max(maxep.get(k[0], -1), k[1])
                            ins = o.fn(e)
                            if o.signal:
                                ins.then_inc(sems[o.sem], 16 if o.dma else 1)
                            tr.append((wl, (o.sem, 16 if o.dma else 1) if o.signal else None, o.idx))
                        fin = {}
                        for o in lst:
                            if o.dma:
                                fin[o.sem] = max(fin.get(o.sem, 0), o.count)
                        for k in sorted(fin):
                            if waited.get(k, 0) < fin[k]:
                                e.wait_ge(sems[k], fin[k])
                    return body
                for eng in ["sp", "act", "pool", "dve", "pe"]:
                    if self.q[eng]:
                        getattr(block, engobj[eng])(make(eng))


def check_protocol(trace):
    sem = {}
    pc = {e: 0 for e in trace}
    progress = True
    while progress:
        progress = False
        for e, tr in trace.items():
            while pc[e] < len(tr):
                wl, sig, idx = tr[pc[e]]
                if all(sem.get(k, 0) >= c for k, c in wl):
                    if sig is not None:
                        sem[sig[0]] = sem.get(sig[0], 0) + sig[1]
                    pc[e] += 1
                    progress = True
                else:
                    break
    stuck = {e: (pc[e], len(tr)) for e, tr in trace.items() if pc[e] < len(tr)}
    return stuck, sem


class Arena:
    LO = 16512
    HI = 229344

    def __init__(self, nc):
        self.nc = nc
        self.top = self.LO
        self.n = 0
        self.peak = 0

    def alloc(self, name, shape, dt):
        esz = 4 if dt == F32 else 2
        nbytes = esz
        for s in shape[1:]:
            nbytes *= s
        off = (self.top + 63) // 64 * 64
        if off + nbytes > self.HI:
            raise RuntimeError("SBUF arena overflow allocating %s %s: off=%d need=%d" % (name, shape, off, nbytes))
        self.top = off + nbytes
        self.peak = max(self.peak, self.top)
        self.n += 1
        return self.nc.alloc_sbuf_tensor_at("%s_%d" % (name, self.n), list(shape), dt, offset=off)

    def mark(self):
        return self.top

    def release(self, m):
        self.top = m


class Ctx:
    pass


def build(n_sub=12, debug=(), parts=None, small_ffn=False):
    nc = bass.Bass("TRN2", target_bir_lowering=False)
    c = Ctx()
    c.nc = nc
    c.P = P = Prog(nc)
    c.A = A = Arena(nc)
    c.debug = debug
    c.dbg_out = {}
    c.parts = parts

    def din(name, shape, dt=F32):
        return nc.dram_tensor(name, list(shape), dt, kind="ExternalInput").ap()

    c.x = din("x", [S, D])
    c.vecs_d = din("vecs", [128, NV])
    c.gateb_d = din("gateb", [4, 8])
    c.ada_w = din("ada_w", [NL, D, 9 * D])
    if small_ffn:
        c.ffn_up = din("ffn_up", [1, 1, 128, 128])
        c.ffn_down = din("ffn_down", [1, 1, 128, 128])
    else:
        c.ffn_up = din("ffn_up", [NL, 2, D, 2 * DFF])
        c.ffn_down = din("ffn_down", [NL, 2, DFF, D])
    c.mix_in = din("mix_in_w", [NL, D, INW])
    c.mix_out = din("mix_out_w", [NL, D, D])
    c.pool_w = din("pool_w", [NL, 4, 64, 64])
    c.qkv_w = din("qkv_w", [NL, 3, 4, 128, 128])
    c.out = nc.dram_tensor("out", [S, D], F32, kind="ExternalOutput").ap()
    c.hT = nc.dram_tensor("hT", [D, S], F32, kind="Internal").ap()
    c.mixT = nc.dram_tensor("mixT", [D, S], BF16, kind="Internal").ap()
    c.BhT = [Buf("hT%d" % i) for i in range(S // 256)]
    c.BmixT = [Buf("mixT%d" % i) for i in range(8)]
    c.ps = nc.alloc_psum_tensor("ps", [128, 8, 512], F32)
    c.Bps = [Buf("ps%d" % i, excl=True) for i in range(8)]
    c.rr = 0

    def bank():
        i = c.rr % 8
        c.rr += 1
        return i
    c.bank = bank

    def dbg(name, shape, dt=F32):
        t = nc.dram_tensor("dbg_" + name, list(shape), dt, kind="ExternalOutput").ap()
        c.dbg_out[name] = t
        return t
    c.dbg = dbg

    prologue(c)
    P.barrier()
    t_in(c)
    nsub = 0
    for l in range(NL):
        for j in range(3):
            if nsub >= n_sub:
                break
            P.barrier()
            if j == 1:
                mixer_phase(c, l)
            elif not small_ffn:
                ffn_phase(c, l, j)
            nsub += 1
    P.barrier()
    t_out(c)
    P.emit()
    stuck, semv = check_protocol(P.trace)
    if stuck:
        raise RuntimeError("protocol deadlock: %s" % stuck)
    c.maxsem = max(semv.values())
    c.stats = dict(nops=len(P.ops), nsem=P.nsem, nwaits=P.nwaits, peak=A.peak,
                   per_eng={k: len(v) for k, v in P.q.items()})
    return nc, c


def hT_tile(c, t0, tt):
    return c.hT.rearrange("(dc p) t -> p dc t", p=128)[:, :, t0:t0 + tt]


def hT_bufs(c, t0, tt):
    return c.BhT[t0 // 256:(t0 + tt + 255) // 256]


def prologue(c):
    nc, P, A = c.nc, c.P, c.A
    c.vecs = A.alloc("vecs", [128, NV], F32)
    c.Bvecs = Buf("vecs")
    P.dma("sp", c.vecs[:], c.vecs_d, writes=[c.Bvecs])
    c.gateb = A.alloc("gateb", [4, 8], F32)
    c.Bgateb = Buf("gateb")
    P.dma("sp", c.gateb[:], c.gateb_d, writes=[c.Bgateb])
    c.ones_bf = A.alloc("ones", [128, 128], BF16)
    c.Bones = Buf("ones")
    P.dve(lambda e: e.memset(c.ones_bf[:], 1.0), writes=[c.Bones])
    c.ident = A.alloc("ident", [128, 128], F32)
    c.Bident = Buf("ident")
    P.pool(lambda e: e.memset(c.ident[:], 1.0), writes=[c.Bident])
    P.pool(lambda e: e.affine_select(out=c.ident[:], in_=c.ident[:], pattern=[[-1, 128]], compare_op=ALU.is_equal,
                                     fill=0.0, base=0, channel_multiplier=1), reads=[c.Bident], writes=[c.Bident])
    c.selh = A.alloc("selh", [4, 4, 128], F32)
    c.Bselh = Buf("selh")
    P.pool(lambda e: e.memset(c.selh[:], 1.0), writes=[c.Bselh])
    P.pool(lambda e: e.affine_select(out=c.selh[:], in_=c.selh[:], pattern=[[-1, 4], [0, 128]], compare_op=ALU.is_equal,
                                     fill=0.0, base=0, channel_multiplier=1), reads=[c.Bselh], writes=[c.Bselh])
    mk = A.alloc("mk32", [128, 512], F32)
    Bmk = Buf("mk32")
    c.amask = A.alloc("amask", [128, 512], BF16)
    c.Bamask = Buf("amask")
    P.pool(lambda e: e.memset(mk[:], 1.0), writes=[Bmk])
    for i in range(4):
        sl = mk[:, i * 128:(i + 1) * 128]
        if i % 2 == 0:
            P.pool(lambda e, sl=sl: e.affine_select(out=sl, in_=sl, pattern=[[-1, 128]], compare_op=ALU.is_ge, fill=0.0,
                                                    base=0, channel_multiplier=1), reads=[Bmk], writes=[Bmk])
        else:
            P.pool(lambda e, sl=sl: e.affine_select(out=sl, in_=sl, pattern=[[1, 128]], compare_op=ALU.is_ge, fill=0.0,
                                                    base=0, channel_multiplier=-1), reads=[Bmk], writes=[Bmk])
    P.dve(lambda e: e.tensor_copy(out=c.amask[:], in_=mk[:]), reads=[Bmk], writes=[c.Bamask])
    mk2 = A.alloc("mk2", [128, 128], F32)
    Bmk2 = Buf("mk2")
    c.lmask = A.alloc("lmask", [128, 128], BF16)
    c.Blmask = Buf("lmask")
    P.pool(lambda e: e.memset(mk2[:], 1.0), writes=[Bmk2])
    P.pool(lambda e: e.affine_select(out=mk2[:], in_=mk2[:], pattern=[[1, 128]], compare_op=ALU.is_ge, fill=0.0,
                                     base=0, channel_multiplier=-1), reads=[Bmk2], writes=[Bmk2])
    P.pool(lambda e: e.memset(mk2[0:64, 64:128], 0.0), reads=[Bmk2], writes=[Bmk2])
    P.dve(lambda e: e.tensor_copy(out=c.lmask[:], in_=mk2[:]), reads=[Bmk2], writes=[c.Blmask])
    c.invw = A.alloc("invw", [128, 2], F32)
    c.invcnt = A.alloc("invcnt", [128, 2, 16], F32)
    c.Bpoolc = Buf("poolc")
    wins = (2, 4, 8, 16)
    first = True
    for pc in range(2):
        for half in range(2):
            w = wins[pc * 2 + half]
            pr = slice(half * 64, half * 64 + 64)
            P.pool(lambda e, pr=pr, pc=pc, w=w: e.memset(c.invw[pr, pc:pc + 1], 1.0 / w), reads=[c.Bpoolc], writes=[c.Bpoolc])
            for t in range(16):
                P.pool(lambda e, pr=pr, pc=pc, w=w, t=t: e.memset(c.invcnt[pr, pc, t:t + 1], 1.0 / min(t + 1, w)),
                       reads=[c.Bpoolc], writes=[c.Bpoolc])
    c.cact = A.alloc("cact", [128, 8], BF16)
    c.Bcact = Buf("cact")
    P.act(lambda e: e.activation(out=c.cact[:], in_=c.vecs[:, CT:CT + 8], func=AF.Silu), reads=[c.Bvecs], writes=[c.Bcact])
    c.modT = A.alloc("modT", [128, NL * 72], F32)
    c.Bmod = Buf("modT")
    m = A.mark()
    NPC = 8
    PW = 9 * D // NPC
    pieces = [A.alloc("adap", [128, 8, PW], BF16) for _ in range(2)]
    Bpieces = [Buf("adap0"), Buf("adap1")]
    mb = c.bank()
    k = 0
    for l in range(NL):
        src = c.ada_w[l].rearrange("(kc p) n -> p kc n", p=128)
        for pc in range(NPC):
            pt, Bp = pieces[k % 2], Bpieces[k % 2]
            k += 1
            P.dma("pool", pt[:], src[:, :, pc * PW:(pc + 1) * PW], writes=[Bp])
            for ocl in range(PW // 128):
                oc = pc * (PW // 128) + ocl
                col = l * 72 + oc
                for kc in range(8):
                    P.pe(lambda e, pt=pt, kc=kc, ocl=ocl, col=col: e.matmul(
                        c.ps[:, mb, col:col + 1], pt[:, kc, ocl * 128:(ocl + 1) * 128], c.cact[:, kc:kc + 1],
                        start=(kc == 0), stop=(kc == 7)), reads=[Bp, c.Bcact], writes=[c.Bps[mb]])
    P.dve(lambda e: e.tensor_tensor(out=c.modT[:], in0=c.ps[:, mb, 0:NL * 72], in1=c.vecs[:, ADAB:ADAB + NL * 72], op=ALU.add),
          reads=[c.Bps[mb], c.Bvecs], writes=[c.Bmod])
    A.release(m)
    c.Avec = A.alloc("Avec", [128, NL * 3 * 8], F32)
    c.Gvec = A.alloc("Gvec", [128, NL * 3 * 8], F32)
    c.Bder = Buf("der")
    for l in range(NL):
        for j in range(3):
            o = (l * 3 + j) * 8
            sc = c.modT[:, l * 72 + (j * 3 + 1) * 8: l * 72 + (j * 3 + 1) * 8 + 8]
            gt = c.modT[:, l * 72 + (j * 3 + 2) * 8: l * 72 + (j * 3 + 2) * 8 + 8]
            wgt = 1.0 if j == 1 else 0.5
            P.dve(lambda e, o=o, sc=sc: e.scalar_tensor_tensor(out=c.Avec[:, o:o + 8], in0=sc, scalar=1.0,
                                                               in1=c.vecs[:, PREW + o:PREW + o + 8], op0=ALU.add, op1=ALU.mult),
                  reads=[c.Bmod, c.Bvecs], writes=[c.Bder])
            P.dve(lambda e, o=o, gt=gt, wgt=wgt: e.scalar_tensor_tensor(out=c.Gvec[:, o:o + 8], in0=gt, scalar=wgt,
                                                                        in1=c.vecs[:, POSTW + o:POSTW + o + 8], op0=ALU.mult, op1=ALU.mult),
                  reads=[c.Bmod, c.Bvecs], writes=[c.Bder])
    if "mod" in c.debug:
        d = c.dbg("mod", [128, NL * 72])
        P.dma("sp", d, c.modT[:], reads=[c.Bmod])


def shift_ap(c, l, j, dc):
    o = l * 72 + (j * 3 + 0) * 8 + dc
    return c.modT[:, o:o + 1]


def A_ap(c, l, j, dc):
    o = (l * 3 + j) * 8 + dc
    return c.Avec[:, o:o + 1]


def G_ap(c, l, j, dc):
    o = (l * 3 + j) * 8 + dc
    return c.Gvec[:, o:o + 1]


def t_in(c):
    nc, P, A = c.nc, c.P, c.A
    m = A.mark()
    NX = 8
    xt = [A.alloc("xt", [128, D], F32) for _ in range(NX)]
    Bxt = [Buf("xt%d" % i) for i in range(NX)]
    hst = [A.alloc("hst", [128, 8, 512], F32) for _ in range(2)]
    Bhst = [Buf("hst%d" % i) for i in range(2)]
    ev = 0
    for g in range(S // 512):
        hs, Bh = hst[g % 2], Bhst[g % 2]
        for half in range(2):
            banks = [c.bank() for _ in range(4)]
            for tt in range(4):
                xi = (g * 4 + tt) % NX
                if half == 0:
                    r0 = g * 512 + tt * 128
                    P.dma("sp", xt[xi][:], c.x[r0:r0 + 128, :], writes=[Bxt[xi]])
                for d in range(4):
                    dc = half * 4 + d
                    P.pe(lambda e, b=banks[d], tt=tt, xi=xi, dc=dc: e.transpose(
                        c.ps[:, b, tt * 128:(tt + 1) * 128], xt[xi][:, dc * 128:(dc + 1) * 128], c.ident[:]),
                        reads=[Bxt[xi], c.Bident], writes=[c.Bps[banks[d]]])
            for d in range(4):
                dc = half * 4 + d
                b = banks[d]
                if ev % 2 == 0:
                    P.act(lambda e, b=b, dc=dc, hs=hs: e.copy(out=hs[:, dc, :], in_=c.ps[:, b, :]), reads=[c.Bps[b]], writes=[Bh])
                else:
                    P.dve(lambda e, b=b, dc=dc, hs=hs: e.tensor_copy(out=hs[:, dc, :], in_=c.ps[:, b, :]), reads=[c.Bps[b]], writes=[Bh])
                ev += 1
        P.dma("sp", hT_tile(c, g * 512, 512), hs[:], reads=[Bh], writes=hT_bufs(c, g * 512, 512))
    A.release(m)


def t_out(c):
    nc, P, A = c.nc, c.P, c.A
    m = A.mark()
    hst = [A.alloc("hst", [128, 8, 512], F32) for _ in range(2)]
    Bhst = [Buf("ohst%d" % i) for i in range(2)]
    NO = 4
    ot = [A.alloc("ot", [128, D], F32) for _ in range(NO)]
    Bot = [Buf("ot%d" % i) for i in range(NO)]
    k = 0
    for g in range(S // 512):
        hs, Bh = hst[g % 2], Bhst[g % 2]
        P.dma("sp", hs[:], hT_tile(c, g * 512, 512), reads=hT_bufs(c, g * 512, 512), writes=[Bh])
        for tt in range(4):
            o, Bo = ot[k % NO], Bot[k % NO]
            k += 1
            for half in range(2):
                b = c.bank()
                for d in range(4):
                    dc = half * 4 + d
                    P.pe(lambda e, b=b, d=d, dc=dc, hs=hs, tt=tt: e.transpose(
                        c.ps[:, b, d * 128:(d + 1) * 128], hs[:, dc, tt * 128:(tt + 1) * 128], c.ident[:]),
                        reads=[Bh, c.Bident], writes=[c.Bps[b]])
                if half == 0:
                    P.act(lambda e, b=b, o=o: e.copy(out=o[:, 0:512], in_=c.ps[:, b, :]), reads=[c.Bps[b]], writes=[Bo])
                else:
                    P.dve(lambda e, b=b, o=o: e.tensor_copy(out=o[:, 512:1024], in_=c.ps[:, b, :]), reads=[c.Bps[b]], writes=[Bo])
            r0 = g * 512 + tt * 128
            P.dma("sp", c.out[r0:r0 + 128, :], o[:], reads=[Bo])
    A.release(m)


class NormBufs:
    def __init__(self, c, TT, name):
        A = c.A
        self.TT = TT
        self.sq = [A.alloc(name + "sq", [128, 8, TT], BF16) for _ in range(2)]
        self.Bsq = [Buf(name + "sq%d" % i) for i in range(2)]
        self.rs = [A.alloc(name + "rs", [128, TT], F32) for _ in range(2)]
        self.Brs = [Buf(name + "rs%d" % i) for i in range(2)]
        self.tmp = A.alloc(name + "tmp", [128, 8, TT], F32)
        self.Btmp = Buf(name + "tmp")
        self.k = 0


def sumsq_rstd(c, nb, src, Bsrc):
    P = c.P
    TT = nb.TT
    i = nb.k % 2
    nb.k += 1
    sq, Bsq, rs, Brs = nb.sq[i], nb.Bsq[i], nb.rs[i], nb.Brs[i]
    P.pool(lambda e: e.tensor_tensor(out=sq[:], in0=src, in1=src, op=ALU.mult), reads=[Bsrc], writes=[Bsq])
    b = c.bank()
    for dc in range(8):
        P.pe(lambda e, dc=dc: e.matmul(c.ps[:, b, 0:TT], c.ones_bf[:], sq[:, dc, :], start=(dc == 0), stop=(dc == 7)),
             reads=[Bsq, c.Bones], writes=[c.Bps[b]])
    P.act(lambda e: e.activation(out=rs[:], in_=c.ps[:, b, 0:TT], func=AF.Sqrt, scale=1.0 / D, bias=EPS),
          reads=[c.Bps[b]], writes=[Brs])
    P.dve(lambda e: e.reciprocal(out=rs[:], in_=rs[:]), reads=[Brs], writes=[Brs])
    return rs, Brs


def pre_stage(c, nb, ht, Bht, l, j, uT_dst, BuT):
    P = c.P
    TT = nb.TT
    rs, Brs = sumsq_rstd(c, nb, ht, Bht)
    P.dve(lambda e: e.tensor_tensor(out=nb.tmp[:], in0=ht, in1=rs[:, :].unsqueeze(1).broadcast_to([128, 8, TT]), op=ALU.mult),
          reads=[Bht, Brs], writes=[nb.Btmp])
    for dc in range(8):
        P.act(lambda e, dc=dc: e.activation(out=uT_dst[:, dc, :], in_=nb.tmp[:, dc, :], func=AF.Identity,
                                            scale=A_ap(c, l, j, dc), bias=shift_ap(c, l, j, dc)),
              reads=[nb.Btmp, c.Bder, c.Bmod], writes=[BuT])


def post_stage(c, nb, y, By, ht, Bht, l, j):
    P = c.P
    TT = nb.TT
    rs, Brs = sumsq_rstd(c, nb, y, By)
    P.dve(lambda e: e.tensor_tensor(out=nb.tmp[:], in0=y, in1=rs[:, :].unsqueeze(1).broadcast_to([128, 8, TT]), op=ALU.mult),
          reads=[By, Brs], writes=[nb.Btmp])
    for dc in range(8):
        P.dve(lambda e, dc=dc: e.scalar_tensor_tensor(out=ht[:, dc, :], in0=nb.tmp[:, dc, :], scalar=G_ap(c, l, j, dc),
                                                      in1=ht[:, dc, :], op0=ALU.mult, op1=ALU.add),
              reads=[nb.Btmp, c.Bder, Bht], writes=[Bht])


def ffn_phase(c, l, j):
    nc, P, A = c.nc, c.P, c.A
    fi = 0 if j == 0 else 1
    m = A.mark()
    TT = 256
    NT = S // TT
    NKF = DFF // 128
    Wup = A.alloc("Wup", [128, 8, 2 * DFF], BF16)
    BWup = [Buf("Wup%d" % k) for k in range(8)]
    Wdn = A.alloc("Wdn", [128, NKF, D], BF16)
    BWdn = [Buf("Wdn%d" % k) for k in range(2)]
    for kc in range(8):
        P.dma("pool", Wup[:, kc, :], c.ffn_up[l, fi, kc * 128:(kc + 1) * 128, :], writes=[BWup[kc]])
    dsrc = c.ffn_down[l, fi].rearrange("(kc p) n -> p kc n", p=128)
    for pc in range(2):
        P.dma("pool", Wdn[:, pc * 11:(pc + 1) * 11, :], dsrc[:, pc * 11:(pc + 1) * 11, :], writes=[BWdn[pc]])
    nb = NormBufs(c, TT, "f")
    ht = [A.alloc("ht", [128, 8, TT], F32) for _ in range(2)]
    Bht = [Buf("ht%d" % i) for i in range(2)]
    uT = [A.alloc("uT", [128, 8, TT], BF16) for _ in range(2)]
    BuT = [Buf("uT%d" % i) for i in range(2)]
    sg = [A.alloc("sg", [128, TT], F32) for _ in range(3)]
    Bsg = [Buf("sg%d" % i) for i in range(3)]
    hid = A.alloc("hid", [128, NKF, TT], BF16)
    Bhid = [Buf("hid%d" % k) for k in range(NKF)]
    y = A.alloc("y", [128, 8, TT], F32)
    By = Buf("y")

    def load(i):
        P.dma("sp", ht[i % 2][:], hT_tile(c, i * TT, TT), reads=hT_bufs(c, i * TT, TT), writes=[Bht[i % 2]])

    def pre(i):
        pre_stage(c, nb, ht[i % 2][:], Bht[i % 2], l, j, uT[i % 2], BuT[i % 2])

    sgk = [0]

    def up(i):
        u = uT[i % 2]
        for jj in range(NKF):
            b = c.bank()
            for half in range(2):
                col0 = half * DFF + jj * 128
                for kc in range(8):
                    P.pe(lambda e, b=b, half=half, col0=col0, kc=kc, u=u: e.matmul(
                        c.ps[:, b, half * TT:(half + 1) * TT], Wup[:, kc, col0:col0 + 128], u[:, kc, :],
                        start=(kc == 0), stop=(kc == 7)), reads=[BWup[kc], BuT[i % 2]], writes=[c.Bps[b]])
            si = sgk[0] % 3
            sgk[0] += 1
            P.act(lambda e, b=b, si=si: e.activation(out=sg[si][:], in_=c.ps[:, b, 0:TT], func=AF.Silu),
                  reads=[c.Bps[b]], writes=[Bsg[si]])
            P.dve(lambda e, b=b, si=si, jj=jj: e.tensor_tensor(out=hid[:, jj, :], in0=sg[si][:], in1=c.ps[:, b, TT:2 * TT], op=ALU.mult),
                  reads=[c.Bps[b], Bsg[si]], writes=[Bhid[jj]])

    def down(i):
        for dc in range(8):
            b = c.bank()
            for kc in range(NKF):
                P.pe(lambda e, b=b, dc=dc, kc=kc: e.matmul(
                    c.ps[:, b, 0:TT], Wdn[:, kc, dc * 128:(dc + 1) * 128], hid[:, kc, :],
                    start=(kc == 0), stop=(kc == NKF - 1)), reads=[BWdn[kc // 11], Bhid[kc]], writes=[c.Bps[b]])
            P.act(lambda e, b=b, dc=dc: e.copy(out=y[:, dc, :], in_=c.ps[:, b, 0:TT]), reads=[c.Bps[b]], writes=[By])

    def post(i):
        post_stage(c, nb, y[:], By, ht[i % 2][:], Bht[i % 2], l, j)
        P.dma("sp", hT_tile(c, i * TT, TT), ht[i % 2][:], reads=[Bht[i % 2]], writes=hT_bufs(c, i * TT, TT))

    load(0)
    pre(0)
    for i in range(NT):
        if i + 1 < NT:
            load(i + 1)
        up(i)
        if i + 1 < NT:
            pre(i + 1)
        down(i)
        post(i)
    A.release(m)


def mixer_phase(c, l):
    nc, P, A = c.nc, c.P, c.A
    m = A.mark()
    uT = A.alloc("uTall", [128, 8, S], BF16)
    BuT = [Buf("uTall%d" % g) for g in range(8)]
    Win = A.alloc("Win", [128, 8, INW], BF16)
    BWin = Buf("Win")
    P.dma("pool", Win[:], c.mix_in[l].rearrange("(kc p) n -> p kc n", p=128), writes=[BWin])
    m0 = A.mark()
    TT = 512
    nb = NormBufs(c, TT, "m")
    ht = [A.alloc("mht", [128, 8, TT], F32) for _ in range(2)]
    Bht = [Buf("mht%d" % i) for i in range(2)]
    for g in range(S // TT):
        P.dma("sp", ht[g % 2][:], hT_tile(c, g * TT, TT), reads=hT_bufs(c, g * TT, TT), writes=[Bht[g % 2]])
        pre_stage(c, nb, ht[g % 2][:], Bht[g % 2], l, 1, uT[:, :, g * TT:(g + 1) * TT], BuT[g])
    A.release(m0)
    P.barrier()
    if "uT" in c.debug and l == 0:
        d = c.dbg("uT", [128, 8, S], BF16)
        P.dma("sp", d, uT[:], reads=BuT)
    mctx = Ctx()
    mctx.uT, mctx.BuT, mctx.Win, mctx.BWin = uT, BuT, Win, BWin
    pp = c.parts
    if pp is None or "att" in pp:
        attention(c, l, mctx)
        P.barrier()
    if pp is None or "pool" in pp:
        pool_mixer(c, l, mctx)
        P.barrier()
    if pp is None or "mlstm" in pp or "gates" in pp:
        mlstm(c, l, mctx)
        P.barrier()
    A.release(m)
    if pp is None or "outproj" in pp:
        outproj(c, l)


def inproj_fm(c, mctx, col0, ncols, g, b, boff=0, prow=None):
    P = c.P
    for kc in range(8):
        P.pe(lambda e, kc=kc: e.matmul(c.ps[0:ncols, b, boff:boff + 512], mctx.Win[:, kc, col0:col0 + ncols],
                                       mctx.uT[:, kc, g * 512:(g + 1) * 512], start=(kc == 0), stop=(kc == 7)),
             reads=[mctx.BWin, mctx.BuT[g]], writes=[c.Bps[b]])


def attention(c, l, mctx):
    nc, P, A = c.nc, c.P, c.A
    m = A.mark()
    uT, BuT, Win, BWin = mctx.uT, mctx.BuT, mctx.Win, mctx.BWin
    qT = A.alloc("qT", [128, S], BF16)
    kT = A.alloc("kT", [128, S], BF16)
    Bq, Bk = Buf("qT"), Buf("kT")
    ve = A.alloc("ve", [128, 32, 68], BF16)
    vo = A.alloc("vo", [128, 32, 128], BF16)
    Bve, Bvo = Buf("ve"), Buf("vo")
    acc_e = A.alloc("acce", [65, S], F32)
    acc_o = A.alloc("acco", [128, S], F32)
    Bacc = [Buf("acc%d" % g) for g in range(8)]
    pT = [A.alloc("pT", [128, 512], BF16) for _ in range(3)]
    BpT = [Buf("pT%d" % i) for i in range(3)]
    rec = A.alloc("rec", [128, 512], F32)
    Brec = Buf("rec")
    ost = [A.alloc("ost", [128, 512], BF16) for _ in range(2)]
    Bost = [Buf("ost%d" % i) for i in range(2)]
    sele = A.alloc("sele", [65, 128], F32)
    selo = A.alloc("selo", [128, 128], F32)
    Bsel = Buf("sel")
    P.pool(lambda e: e.memset(sele[:], 0.0), writes=[Bsel])
    P.pool(lambda e: e.memset(selo[:], 0.0), reads=[Bsel], writes=[Bsel])
    P.pool(lambda e: e.memset(sele[64:65, 0:64], 1.0), reads=[Bsel], writes=[Bsel])
    P.pool(lambda e: e.memset(selo[0:1, 64:128], 1.0), reads=[Bsel], writes=[Bsel])
    pk = [0]
    ok = [0]
    branches = ((128, 1), (512, 4), (2048, 16))
    for pair in range(2):
        for g in range(8):
            b = c.bank()
            inproj_fm(c, mctx, pair * 128, 128, g, b)
            P.act(lambda e, b=b, g=g: e.copy(out=qT[:, g * 512:(g + 1) * 512], in_=c.ps[:, b, :]), reads=[c.Bps[b]], writes=[Bq])
            b = c.bank()
            inproj_fm(c, mctx, 256 + pair * 128, 128, g, b)
            P.dve(lambda e, b=b, g=g: e.tensor_copy(out=kT[:, g * 512:(g + 1) * 512], in_=c.ps[:, b, :]), reads=[c.Bps[b]], writes=[Bk])
        P.pool(lambda e: e.memset(ve[:, :, 64:65], 1.0), reads=[Bve], writes=[Bve])
        P.pool(lambda e: e.memset(vo[:, :, 0:64], 0.0), reads=[Bvo], writes=[Bvo])
        P.pool(lambda e: e.memset(vo[:, :, 0:1], 1.0), reads=[Bvo], writes=[Bvo])
        for bi, (win, dil) in enumerate(branches):
            L = S // dil
            nbk = L // 128

            def tok(r, n, dil=dil):
                start = r + dil * 128 * n
                return slice(start, start + dil * 127 + 1, dil)
            blocks = [(r, n) for n in range(nbk) for r in range(dil)]
            if dil == 1:
                blocks = [(0, n) for n in range(nbk)]
            for q4 in range(8):
                b = c.bank()
                for i4 in range(4):
                    bl = q4 * 4 + i4
                    r, n = blocks[bl]
                    for kc in range(8):
                        P.pe(lambda e, b=b, i4=i4, kc=kc, ts=tok(r, n): e.matmul(
                            c.ps[:, b, i4 * 128:(i4 + 1) * 128], uT[:, kc, ts], Win[:, kc, 512 + pair * 128: 512 + pair * 128 + 128],
                            start=(kc == 0), stop=(kc == 7)), reads=BuT + [BWin], writes=[c.Bps[b]])
                psv = c.ps[:, b, :].rearrange("p (i f) -> p i f", f=128)
                P.act(lambda e, psv=psv, q4=q4: e.copy(out=ve[:, q4 * 4:(q4 + 1) * 4, 0:64], in_=psv[:, :, 0:64]),
                      reads=[c.Bps[b]], writes=[Bve])
                P.dve(lambda e, psv=psv, q4=q4: e.tensor_copy(out=vo[:, q4 * 4:(q4 + 1) * 4, 64:128], in_=psv[:, :, 64:128]),
                      reads=[c.Bps[b]], writes=[Bvo])
            blk_index = {rn: i for i, rn in enumerate(blocks)}
            for hh in range(2):
                pr = slice(hh * 64, hh * 64 + 64)
                vx, Bvx, M = (ve, Bve, 65) if hh == 0 else (vo, Bvo, 128)
                vcol = slice(0, M)
                acc = acc_e if hh == 0 else acc_o
                for q4 in range(8):
                    bo = c.bank()
                    for i2 in range(2):
                        bs = c.bank()
                        blist = [blocks[q4 * 4 + i2 * 2 + t] for t in range(2)]
                        for t, (r, n) in enumerate(blist):
                            qs = tok(r, n)
                            if True:
                                P.pe(lambda e, bs=bs, t=t, ks=tok(r, max(n - 1, 0)), qs=qs: e.matmul(
                                    c.ps[:, bs, (2 * t) * 128:(2 * t + 1) * 128], kT[pr, ks], qT[pr, qs], start=True, stop=True),
                                    reads=[Bq, Bk], writes=[c.Bps[bs]])
                            P.pe(lambda e, bs=bs, t=t, qs=qs: e.matmul(
                                c.ps[:, bs, (2 * t + 1) * 128:(2 * t + 2) * 128], kT[pr, qs], qT[pr, qs], start=True, stop=True),
                                reads=[Bq, Bk], writes=[c.Bps[bs]])
                        pi = pk[0] % 3
                        pk[0] += 1
                        p_t, Bp = pT[pi], BpT[pi]
                        P.act(lambda e, bs=bs, p_t=p_t: e.activation(out=p_t[:], in_=c.ps[:, bs, :], func=AF.Exp, scale=0.125),
                              reads=[c.Bps[bs]], writes=[Bp])
                        P.pool(lambda e, p_t=p_t: e.tensor_tensor(out=p_t[:], in0=p_t[:], in1=c.amask[:], op=ALU.mult),
                               reads=[Bp, c.Bamask], writes=[Bp])
                        for t, (r, n) in enumerate(blist):
                            i4 = i2 * 2 + t
                            bl = blk_index[(r, n)]
                            if n >= 1:
                                blp = blk_index[(r, n - 1)]
                                P.pe(lambda e, bo=bo, i4=i4, blp=blp, t=t, p_t=p_t, vx=vx, M=M: e.matmul(
                                    c.ps[0:M, bo, i4 * 128:(i4 + 1) * 128], vx[:, blp, 0:M], p_t[:, (2 * t) * 128:(2 * t + 1) * 128],
                                    start=True, stop=False), reads=[Bvx, Bp], writes=[c.Bps[bo]])
                            P.pe(lambda e, bo=bo, i4=i4, bl=bl, t=t, p_t=p_t, vx=vx, M=M, n=n: e.matmul(
                                c.ps[0:M, bo, i4 * 128:(i4 + 1) * 128], vx[:, bl, 0:M], p_t[:, (2 * t + 1) * 128:(2 * t + 2) * 128],
                                start=(n == 0), stop=True), reads=[Bvx, Bp], writes=[c.Bps[bo]])
                    r0, n0 = blocks[q4 * 4]
                    if dil == 1:
                        t0 = n0 * 128
                        av = acc[0:M, t0:t0 + 512]
                        pv = c.ps[0:M, bo, :]
                        gset = [Bacc[t0 // 512]]
                    elif dil == 4:
                        t0 = 512 * n0
                        av = acc[0:M, t0:t0 + 512].rearrange("p (q r) -> p r q", r=4)
                        pv = c.ps[0:M, bo, :].rearrange("p (r q) -> p r q", r=4)
                        gset = [Bacc[t0 // 512]]
                    else:
                        t0 = 2048 * n0
                        av = acc[0:M, t0:t0 + 2048].rearrange("p (q r) -> p r q", r=16)[:, r0:r0 + 4, :]
                        pv = c.ps[0:M, bo, :].rearrange("p (r q) -> p r q", r=4)
                        gset = Bacc[t0 // 512: t0 // 512 + 4]
                    if bi == 0:
                        P.act(lambda e, av=av, pv=pv: e.copy(out=av, in_=pv), reads=[c.Bps[bo]], writes=gset)
                    else:
                        P.dve(lambda e, av=av, pv=pv: e.tensor_tensor(out=av, in0=av, in1=pv, op=ALU.add),
                              reads=[c.Bps[bo]] + gset, writes=gset)
        for g in range(8):
            b = c.bank()
            sl = slice(g * 512, (g + 1) * 512)
            P.pe(lambda e, b=b, sl=sl: e.matmul(c.ps[:, b, :], sele[:], acc_e[:, sl], start=True, stop=False),
                 reads=[Bsel, Bacc[g]], writes=[c.Bps[b]])
            P.pe(lambda e, b=b, sl=sl: e.matmul(c.ps[:, b, :], selo[:], acc_o[:, sl], start=False, stop=True),
                 reads=[Bsel, Bacc[g]], writes=[c.Bps[b]])
            P.dve(lambda e, b=b: e.reciprocal(out=rec[:], in_=c.ps[:, b, :]), reads=[c.Bps[b]], writes=[Brec])
            oi = ok[0] % 2
            ok[0] += 1
            o_t, Bo = ost[oi], Bost[oi]
            P.dve(lambda e, sl=sl, o_t=o_t: e.tensor_tensor(out=o_t[0:64, :], in0=acc_e[0:64, sl], in1=rec[0:64, :], op=ALU.mult),
                  reads=[Bacc[g], Brec], writes=[Bo])
            P.pool(lambda e, sl=sl, o_t=o_t: e.tensor_tensor(out=o_t[64:128, :], in0=acc_o[64:128, sl], in1=rec[64:128, :], op=ALU.mult),
                   reads=[Bacc[g], Brec, Bo], writes=[Bo])
            P.dma("sp", c.mixT[pair * 128:(pair + 1) * 128, sl], o_t[:], reads=[Bo], writes=[c.BmixT[pair]])
    A.release(m)


def pool_mixer(c, l, mctx):
    nc, P, A = c.nc, c.P, c.A
    m = A.mark()
    uT, BuT, Win, BWin = mctx.uT, mctx.BuT, mctx.Win, mctx.BWin
    p32 = A.alloc("p32", [128, S], F32)
    bA = A.alloc("pbA", [128, S], F32)
    bB = A.alloc("pbB", [128, S], F32)
    dT = A.alloc("pdT", [128, S], BF16)
    Bp, BA_, BB_, Bd = Buf("p32"), Buf("pbA"), Buf("pbB"), Buf("pdT")
    PW = A.alloc("PWbd", [128, 128], BF16)
    BPW = Buf("PWbd")
    ost = [A.alloc("post", [128, 512], BF16) for _ in range(2)]
    Bost = [Buf("post%d" % i) for i in range(2)]
    fix = A.alloc("pfix", [128, 16], F32)
    Bfix = Buf("pfix")
    ok = 0
    for pc in range(2):
        P.pool(lambda e: e.memset(PW[:], 0.0), reads=[BPW], writes=[BPW])
        for half in range(2):
            gidx = pc * 2 + half
            P.dma("pool", PW[half * 64:(half + 1) * 64, half * 64:(half + 1) * 64], c.pool_w[l, gidx], reads=[BPW], writes=[BPW])
        for g in range(8):
            b = c.bank()
            inproj_fm(c, mctx, 768 + pc * 128, 128, g, b)
            P.act(lambda e, b=b, g=g: e.copy(out=p32[:, g * 512:(g + 1) * 512], in_=c.ps[:, b, :]), reads=[c.Bps[b]], writes=[Bp])

        def shifted_add(dst, Bdst, src, Bsrc, k):
            P.dve(lambda e: e.tensor_tensor(out=dst[:, k:S], in0=src[:, k:S], in1=src[:, 0:S - k], op=ALU.add),
                  reads=[Bsrc], writes=[Bdst])
            P.act(lambda e: e.copy(out=dst[:, 0:k], in_=src[:, 0:k]), reads=[Bsrc, Bdst], writes=[Bdst])
        shifted_add(bA, BA_, p32, Bp, 1)
        shifted_add(bB, BB_, bA, BA_, 2)
        if pc == 0:
            lo, Blo, hi, Bhi = bA, BA_, bB, BB_
        else:
            shifted_add(bA, BA_, bB, BB_, 4)
            shifted_add(bB, BB_, bA, BA_, 8)
            lo, Blo, hi, Bhi = bA, BA_, bB, BB_
        for half, (ws, Bws) in enumerate(((lo, Blo), (hi, Bhi))):
            pr = slice(half * 64, half * 64 + 64)
            P.dve(lambda e, pr=pr, ws=ws: e.scalar_tensor_tensor(out=dT[pr, :], in0=ws[pr, :], scalar=c.invw[pr, pc:pc + 1],
                                                                 in1=p32[pr, :], op0=ALU.mult, op1=ALU.subtract),
                  reads=[Bws, Bp, c.Bpoolc, Bd], writes=[Bd])
            P.dve(lambda e, pr=pr, ws=ws: e.tensor_tensor(out=fix[pr, :], in0=ws[pr, 0:16], in1=c.invcnt[pr, pc, :], op=ALU.mult),
                  reads=[Bws, c.Bpoolc, Bfix], writes=[Bfix])
            P.dve(lambda e, pr=pr: e.tensor_tensor(out=dT[pr, 0:16], in0=fix[pr, :], in1=p32[pr, 0:16], op=ALU.subtract),
                  reads=[Bfix, Bp, Bd], writes=[Bd])
        for g in range(8):
            b = c.bank()
            sl = slice(g * 512, (g + 1) * 512)
            P.pe(lambda e, b=b, sl=sl: e.matmul(c.ps[:, b, :], PW[:], dT[:, sl], start=True, stop=True),
                 reads=[BPW, Bd], writes=[c.Bps[b]])
            o_t, Bo = ost[ok % 2], Bost[ok % 2]
            ok += 1
            P.act(lambda e, b=b, o_t=o_t: e.activation(out=o_t[:], in_=c.ps[:, b, :], func=AF.Identity,
                                                       scale=c.vecs[:, PSC + l * 2 + pc:PSC + l * 2 + pc + 1]),
                  reads=[c.Bps[b], c.Bvecs], writes=[Bo])
            P.dma("sp", c.mixT[(2 + pc) * 128:(3 + pc) * 128, sl], o_t[:], reads=[Bo], writes=[c.BmixT[2 + pc]])
    A.release(m)


def mlstm(c, l, mctx):
    nc, P, A = c.nc, c.P, c.A
    m = A.mark()
    uT, BuT, Win, BWin = mctx.uT, mctx.BuT, mctx.Win, mctx.BWin
    TOK = A.alloc("TOK", [128, 32, 12], F32)
    BTOK = Buf("TOK")
    DEC = A.alloc("DEC", [128, 4, 64], F32)
    BDEC = Buf("DEC")
    Wqkv = A.alloc("Wqkv", [128, 12, 128], BF16)
    BWq = Buf("Wqkv")
    P.dma("pool", Wqkv[:], c.qkv_w[l].rearrange("q h d e -> d (q h) e"), writes=[BWq])
    mg = A.mark()
    t0 = A.alloc("g0", [4, S], F32)
    t1 = A.alloc("g1", [4, S], F32)
    t2 = A.alloc("g2", [4, S], F32)
    t3 = A.alloc("g3", [4, S], F32)
    t4 = A.alloc("g4", [4, S], F32)
    onesr = A.alloc("g1s", [4, 512], F32)
    B0, B1, B2, B3, B4, Bon = Buf("g0"), Buf("g1"), Buf("g2"), Buf("g3"), Buf("g4"), Buf("g1s")
    rst = A.alloc("rst", [4, 64], F32)
    dec = A.alloc("decr", [4, 64], F32)
    nbf = A.alloc("nbf", [4, 1], F32)
    Brst, Bdec, Bnbf = Buf("rst"), Buf("decr"), Buf("nbf")
    gb = c.gateb
    P.dve(lambda e: e.memset(onesr[:], 1.0), writes=[Bon])
    P.dve(lambda e: e.tensor_scalar(out=nbf[:], in0=gb[:, l * 2 + 1:l * 2 + 2], scalar1=-1.0, scalar2=None, op0=ALU.mult),
          reads=[c.Bgateb], writes=[Bnbf])
    for g in range(8):
        sl = slice(g * 512, (g + 1) * 512)
        b = c.bank()
        inproj_fm(c, mctx, 2048, 4, g, b)
        P.act(lambda e, b=b, sl=sl: e.activation(out=t0[:, sl], in_=c.ps[0:4, b, :], func=AF.Identity,
                                                 bias=gb[:, l * 2:l * 2 + 1], scale=1.0),
              reads=[c.Bps[b], c.Bgateb], writes=[B0])
        b = c.bank()
        inproj_fm(c, mctx, 2052, 4, g, b)
        P.act(lambda e, b=b, sl=sl: e.activation(out=t1[:, sl], in_=c.ps[0:4, b, :], func=AF.Exp, bias=nbf[:], scale=-1.0),
              reads=[c.Bps[b], Bnbf], writes=[B1])
    P.act(lambda e: e.activation(out=t1[:], in_=t1[:], func=AF.Ln, bias=1.0), reads=[B1], writes=[B1])
    for g in range(8):
        sl = slice(g * 512, (g + 1) * 512)
        init = 0.0 if g == 0 else t2[:, g * 512 - 1:g * 512]
        P.dve(lambda e, sl=sl, init=init: e.tensor_tensor_scan(out=t2[:, sl], data0=onesr[:], data1=t1[:, sl], initial=init,
                                                               op0=ALU.mult, op1=ALU.add), reads=[Bon, B1, B2], writes=[B2])
    P.dve(lambda e: e.tensor_tensor(out=t0[:], in0=t0[:], in1=t2[:], op=ALU.add), reads=[B0, B2], writes=[B0])
    for g in range(8):
        sl = slice(g * 512, (g + 1) * 512)
        init = 0.0 if g == 0 else t1[:, g * 512 - 1:g * 512]
        P.dve(lambda e, sl=sl, init=init: e.tensor_tensor_scan(out=t1[:, sl], data0=t0[:, sl], data1=t0[:, sl], initial=init,
                                                               op0=ALU.max, op1=ALU.max), reads=[B0, B1], writes=[B1])
    Rv = t1[:, :].rearrange("p (c l) -> p c l", l=64)
    Rend = Rv[:, :, 63]
    P.dve(lambda e: e.memset(rst[:, 0:1], 0.0), writes=[Brst])
    P.dve(lambda e: e.tensor_copy(out=rst[:, 1:64], in_=Rv[:, 0:63, 63]), reads=[B1, Brst], writes=[Brst])
    P.dve(lambda e: e.tensor_tensor(out=dec[:], in0=rst[:], in1=Rend, op=ALU.subtract), reads=[Brst, B1], writes=[Bdec])
    P.act(lambda e: e.activation(out=dec[:], in_=dec[:], func=AF.Exp), reads=[Bdec], writes=[Bdec])
    Gv = t0[:, :].rearrange("p (c l) -> p c l", l=64)
    Fv = t2[:, :].rearrange("p (c l) -> p c l", l=64)
    rst_b = rst[:, :].unsqueeze(2).broadcast_to([4, 64, 64])
    rend_b = Rv[:, :, 63:64].broadcast_to([4, 64, 64])
    t3v = t3[:, :].rearrange("p (c l) -> p c l", l=64)
    t4v = t4[:, :].rearrange("p (c l) -> p c l", l=64)
    P.dve(lambda e: e.tensor_tensor(out=t3v, in0=Gv, in1=rst_b, op=ALU.subtract), reads=[B0, Brst], writes=[B3])
    P.act(lambda e: e.activation(out=t3[:], in_=t3[:], func=AF.Exp), reads=[B3], writes=[B3])
    P.dve(lambda e: e.tensor_tensor(out=t4v, in0=Gv, in1=rend_b, op=ALU.subtract), reads=[B0, B1], writes=[B4])
    P.act(lambda e: e.activation(out=t4[:], in_=t4[:], func=AF.Exp), reads=[B4], writes=[B4])
    P.dve(lambda e: e.tensor_tensor(out=Gv, in0=Fv, in1=rst_b, op=ALU.subtract), reads=[B2, Brst, B0, B3, B4], writes=[B0])
    P.act(lambda e: e.activation(out=t0[:], in_=t0[:], func=AF.Exp), reads=[B0], writes=[B0])
    for qi, (src, Bs) in enumerate(((t3, B3), (t4, B4), (t0, B0))):
        b = c.bank()
        for tt in range(32):
            P.pe(lambda e, b=b, tt=tt, src=src: e.transpose(c.ps[:, b, tt * 4:(tt + 1) * 4], src[:, tt * 128:(tt + 1) * 128],
                                                            c.ident[0:4, 0:4]), reads=[Bs, c.Bident], writes=[c.Bps[b]])
        P.dve(lambda e, b=b, qi=qi: e.tensor_copy(out=TOK[:, :, qi * 4:(qi + 1) * 4],
                                                   in_=c.ps[:, b, 0:128].rearrange("p (t h) -> p t h", h=4)),
              reads=[c.Bps[b], BTOK], writes=[BTOK])
    b = c.bank()
    for h in range(4):
        P.pe(lambda e, b=b, h=h: e.matmul(c.ps[:, b, h * 64:(h + 1) * 64], c.selh[:, h, :], dec[:], start=True, stop=True),
             reads=[c.Bselh, Bdec], writes=[c.Bps[b]])
    P.dve(lambda e, b=b: e.tensor_copy(out=DEC[:], in_=c.ps[:, b, 0:256].rearrange("p (h c) -> p h c", c=64)),
          reads=[c.Bps[b]], writes=[BDEC])
    if "gates" in c.debug and l == 0:
        d = c.dbg("TOK", [128, 32, 12])
        P.dma("sp", d, TOK[:], reads=[BTOK])
        d = c.dbg("DEC", [128, 4, 64])
        P.dma("sp", d, DEC[:], reads=[BDEC])
    P.barrier()
    A.release(mg)
    if c.parts is not None and "mlstm" not in c.parts:
        A.release(m)
        return
    xmb = A.alloc("xmb", [128, S], BF16)
    acc32 = A.alloc("cacc", [128, S], F32)
    xcb = A.alloc("xcb", [128, S], BF16)
    qA = A.alloc("qA", [128, S], BF16)
    qB = A.alloc("qB", [128, S], BF16)
    kTb = A.alloc("kTb", [128, S], BF16)
    ktok = A.alloc("ktok", [128, 32, 128], BF16)
    va = A.alloc("va", [128, 32, 129], BF16)
    vb = A.alloc("vb", [128, 32, 129], BF16)
    Bxm, Bacc, Bxc, BqA, BqB, BkT, Bkt, Bva, Bvb = [Buf(n) for n in "xmb cacc xcb qA qB kTb ktok va vb".split()]
    C32 = [A.alloc("C32", [128, 129], F32) for _ in range(2)]
    BC32 = [Buf("C32_%d" % i) for i in range(2)]
    NCB = 4
    Cb = [A.alloc("Cb", [128, 129], BF16) for _ in range(NCB)]
    BCb = [Buf("Cb%d" % i) for i in range(NCB)]
    sT = [A.alloc("sT", [128, 128], BF16) for _ in range(2)]
    BsT = [Buf("sT%d" % i) for i in range(2)]
    sog = [A.alloc("sog", [128, 128], F32) for _ in range(2)]
    Bsog = [Buf("sog%d" % i) for i in range(2)]
    hs = [A.alloc("hs", [128, 128], F32) for _ in range(2)]
    Bhs = [Buf("hs%d" % i) for i in range(2)]
    junk = A.alloc("junk", [128, 128], F32)
    Bjunk = Buf("junk")
    sm = [A.alloc("sm", [128, 4], F32) for _ in range(2)]
    Bsm = [Buf("sm%d" % i) for i in range(2)]
    ost = [A.alloc("most", [128, 512], BF16) for _ in range(2)]
    Bost = [Buf("most%d" % i) for i in range(2)]
    ones129 = A.alloc("o129", [128, 32, 1], BF16)
    cwv = lambda tap, h: c.vecs[:, CW + (l * 4 + tap) * 4 + h: CW + (l * 4 + tap) * 4 + h + 1]
    cbv = lambda h: c.vecs[:, CB + l * 4 + h: CB + l * 4 + h + 1]
    nwv = lambda h: c.vecs[:, NW + l * 4 + h: NW + l * 4 + h + 1]
    skv = lambda h: c.vecs[:, SK + l * 4 + h: SK + l * 4 + h + 1]
    qsc = 128.0 ** -0.5
    kk = [0]
    for h in range(4):
        for g in range(8):
            b = c.bank()
            inproj_fm(c, mctx, 1024 + h * 128, 128, g, b)
            P.act(lambda e, b=b, g=g: e.copy(out=xmb[:, g * 512:(g + 1) * 512], in_=c.ps[:, b, :]), reads=[c.Bps[b]], writes=[Bxm])
        P.dve(lambda e, h=h: e.tensor_scalar(out=acc32[:], in0=xmb[:], scalar1=cwv(3, h), scalar2=None, op0=ALU.mult),
              reads=[Bxm, c.Bvecs, Bacc], writes=[Bacc])
        for lag in range(1, 4):
            P.dve(lambda e, h=h, lag=lag: e.scalar_tensor_tensor(out=acc32[:, lag:S], in0=xmb[:, 0:S - lag], scalar=cwv(3 - lag, h),
                                                                 in1=acc32[:, lag:S], op0=ALU.mult, op1=ALU.add),
                  reads=[Bxm, c.Bvecs, Bacc], writes=[Bacc])
        P.act(lambda e, h=h: e.activation(out=xcb[:], in_=acc32[:], func=AF.Silu, bias=cbv(h)), reads=[Bacc, c.Bvecs, Bxc], writes=[Bxc])
        P.pool(lambda e: e.memset(qA[:], 0.0), reads=[BqA], writes=[BqA])
        P.pool(lambda e: e.memset(qB[:], 0.0), reads=[BqB], writes=[BqB])
        for g in range(8):
            sl = slice(g * 512, (g + 1) * 512)
            b = c.bank()
            P.pe(lambda e, b=b, sl=sl, h=h: e.matmul(c.ps[:, b, :], Wqkv[:, 0 * 4 + h, :], xcb[:, sl], start=True, stop=True),
                 reads=[BWq, Bxc], writes=[c.Bps[b]])
            pv = c.ps[:, b, :].rearrange("p (c two l) -> p c two l", two=2, l=64)
            qAv = qA[:, sl].rearrange("p (c two l) -> p c two l", two=2, l=64)
            qBv = qB[:, sl].rearrange("p (c two l) -> p c two l", two=2, l=64)
            P.act(lambda e, pv=pv, qAv=qAv: e.activation(out=qAv[:, :, 0, :], in_=pv[:, :, 0, :], func=AF.Identity, scale=qsc),
                  reads=[c.Bps[b]], writes=[BqA])
            P.dve(lambda e, pv=pv, qBv=qBv: e.tensor_scalar(out=qBv[:, :, 1, :], in0=pv[:, :, 1, :], scalar1=qsc, scalar2=None, op0=ALU.mult),
                  reads=[c.Bps[b]], writes=[BqB])
            b = c.bank()
            P.pe(lambda e, b=b, sl=sl, h=h: e.matmul(c.ps[:, b, :], Wqkv[:, 1 * 4 + h, :], xcb[:, sl], start=True, stop=True),
                 reads=[BWq, Bxc], writes=[c.Bps[b]])
            P.act(lambda e, b=b, sl=sl: e.copy(out=kTb[:, sl], in_=c.ps[:, b, :]), reads=[c.Bps[b]], writes=[BkT])
        for q4 in range(8):
            b = c.bank()
            for i4 in range(4):
                tt = q4 * 4 + i4
                P.pe(lambda e, b=b, i4=i4, tt=tt, h=h: e.matmul(c.ps[:, b, i4 * 128:(i4 + 1) * 128], xcb[:, tt * 128:(tt + 1) * 128],
                                                                 Wqkv[:, 1 * 4 + h, :], start=True, stop=True),
                     reads=[BWq, Bxc], writes=[c.Bps[b]])
            P.act(lambda e, b=b, q4=q4: e.copy(out=ktok[:, q4 * 4:(q4 + 1) * 4, :], in_=c.ps[:, b, :].rearrange("p (i f) -> p i f", f=128)),
                  reads=[c.Bps[b]], writes=[Bkt])
            b = c.bank()
            for i4 in range(4):
                tt = q4 * 4 + i4
                P.pe(lambda e, b=b, i4=i4, tt=tt, h=h: e.matmul(c.ps[:, b, i4 * 128:(i4 + 1) * 128], xmb[:, tt * 128:(tt + 1) * 128],
                                                                 Wqkv[:, 2 * 4 + h, :], start=True, stop=True),
                     reads=[BWq, Bxm], writes=[c.Bps[b]])
            psv = c.ps[:, b, :].rearrange("p (i f) -> p i f", f=128)
            wa_b = TOK[:, q4 * 4:(q4 + 1) * 4, 0 + h:0 + h + 1].broadcast_to([128, 4, 128])
            wb_b = TOK[:, q4 * 4:(q4 + 1) * 4, 4 + h:4 + h + 1].broadcast_to([128, 4, 128])
            P.dve(lambda e, psv=psv, q4=q4, wa_b=wa_b: e.tensor_tensor(out=va[:, q4 * 4:(q4 + 1) * 4, 0:128], in0=psv, in1=wa_b, op=ALU.mult),
                  reads=[c.Bps[b], BTOK], writes=[Bva])
            P.dve(lambda e, psv=psv, q4=q4, wb_b=wb_b: e.tensor_tensor(out=vb[:, q4 * 4:(q4 + 1) * 4, 0:128], in0=psv, in1=wb_b, op=ALU.mult),
                  reads=[c.Bps[b], BTOK], writes=[Bvb])
        P.act(lambda e, h=h: e.copy(out=va[:, :, 128:129], in_=TOK[:, :, 0 + h:0 + h + 1]), reads=[BTOK, Bva], writes=[Bva])
        P.act(lambda e, h=h: e.copy(out=vb[:, :, 128:129], in_=TOK[:, :, 4 + h:4 + h + 1]), reads=[BTOK, Bvb], writes=[Bvb])
        P.act(lambda e, h=h: e.activation(out=xcb[:], in_=xcb[:], func=AF.Identity, scale=skv(h)), reads=[Bxc, c.Bvecs], writes=[Bxc])
        P.dve(lambda e: e.memset(C32[0][:], 0.0), reads=[BC32[0]], writes=[BC32[0]])
        cprev = {}
        ci = 0
        for tt in range(32):
            sl = slice(tt * 128, (tt + 1) * 128)
            b = c.bank()
            P.pe(lambda e, b=b, sl=sl: e.matmul(c.ps[:, b, 0:128], kTb[:, sl], qA[:, sl], start=True, stop=False),
                 reads=[BkT, BqA], writes=[c.Bps[b]])
            P.pe(lambda e, b=b, sl=sl: e.matmul(c.ps[:, b, 0:128], kTb[:, sl], qB[:, sl], start=False, stop=True),
                 reads=[BkT, BqB], writes=[c.Bps[b]])
            si = kk[0] % 2
            kk[0] += 1
            s_t, Bs = sT[si], BsT[si]
            P.dve(lambda e, b=b, s_t=s_t: e.tensor_tensor(out=s_t[:], in0=c.ps[:, b, 0:128], in1=c.lmask[:], op=ALU.mult),
                  reads=[c.Bps[b], c.Blmask], writes=[Bs])
            bg = c.bank()
            for kc in range(8):
                P.pe(lambda e, bg=bg, kc=kc, sl=sl, h=h: e.matmul(c.ps[:, bg, 0:128], uT[:, kc, sl], Win[:, kc, 1536 + h * 128:1536 + (h + 1) * 128],
                                                                 start=(kc == 0), stop=(kc == 7)), reads=BuT + [BWin], writes=[c.Bps[bg]])
            so, Bso = sog[si], Bsog[si]
            P.act(lambda e, bg=bg, so=so: e.activation(out=so[:], in_=c.ps[:, bg, 0:128], func=AF.Sigmoid), reads=[c.Bps[bg]], writes=[Bso])
            for half in range(2):
                ch = tt * 2 + half
                pr = slice(half * 64, half * 64 + 64)
                if ch >= 1:
                    cbt, Bcb = Cb[ci % NCB], BCb[ci % NCB]
                    ci += 1
                    src32, Bsrc = C32[ch % 2], BC32[ch % 2]
                    P.act(lambda e, cbt=cbt, src32=src32: e.copy(out=cbt[:], in_=src32[:]), reads=[Bsrc], writes=[Bcb])
                    cprev[ch] = (cbt, Bcb)
                if ch < 63:
                    bd = c.bank()
                    P.pe(lambda e, bd=bd, pr=pr, tt=tt: e.matmul(c.ps[:, bd, 0:129], ktok[pr, tt, :], vb[pr, tt, :], start=True, stop=True),
                         reads=[Bkt, Bvb], writes=[c.Bps[bd]])
                    cur, Bcur = C32[ch % 2], BC32[ch % 2]
                    nxt, Bnxt = C32[(ch + 1) % 2], BC32[(ch + 1) % 2]
                    P.dve(lambda e, bd=bd, cur=cur, nxt=nxt, ch=ch, h=h: e.scalar_tensor_tensor(
                        out=nxt[:], in0=cur[:], scalar=DEC[:, h, ch:ch + 1], in1=c.ps[:, bd, 0:129], op0=ALU.mult, op1=ALU.add),
                        reads=[Bcur, BDEC, c.Bps[bd]], writes=[Bnxt])
            bo = c.bank()
            mm = [(s_t[:], va[:, tt, :], [Bs, Bva])]
            if tt * 2 in cprev:
                mm.append((qA[:, sl], cprev[tt * 2][0][:], [BqA, cprev[tt * 2][1]]))
            mm.append((qB[:, sl], cprev[tt * 2 + 1][0][:], [BqB, cprev[tt * 2 + 1][1]]))
            for i, (lt, rh, rd) in enumerate(mm):
                P.pe(lambda e, bo=bo, lt=lt, rh=rh, i=i, n=len(mm): e.matmul(c.ps[:, bo, 0:129], lt, rh, start=(i == 0), stop=(i == n - 1)),
                     reads=rd, writes=[c.Bps[bo]])
            sm_t, Bsm_t = sm[si], Bsm[si]
            h_t, Bh_t = hs[si], Bhs[si]
            P.act(lambda e, bo=bo, sm_t=sm_t: e.activation(out=sm_t[:, 0:1], in_=c.ps[:, bo, 128:129], func=AF.Abs),
                  reads=[c.Bps[bo]], writes=[Bsm_t])
            P.dve(lambda e, sm_t=sm_t, tt=tt, h=h: e.tensor_tensor(out=sm_t[:, 0:1], in0=sm_t[:, 0:1], in1=TOK[:, tt, 8 + h:8 + h + 1], op=ALU.max),
                  reads=[Bsm_t, BTOK], writes=[Bsm_t])
            P.dve(lambda e, sm_t=sm_t: e.reciprocal(out=sm_t[:, 1:2], in_=sm_t[:, 0:1]), reads=[Bsm_t], writes=[Bsm_t])
            P.dve(lambda e, bo=bo, sm_t=sm_t, h_t=h_t, so=so: e.scalar_tensor_tensor(out=h_t[:], in0=c.ps[:, bo, 0:128], scalar=sm_t[:, 1:2],
                                                                                     in1=so[:], op0=ALU.mult, op1=ALU.mult),
                  reads=[c.Bps[bo], Bsm_t, Bso], writes=[Bh_t])
            P.dve(lambda e, sm_t=sm_t: e.memset(sm_t[:, 2:3], 0.0), reads=[Bsm_t], writes=[Bsm_t])
            P.act(lambda e, sm_t=sm_t, h_t=h_t: e.activation(out=junk[:], in_=h_t[:], func=AF.Square, accum_out=sm_t[:, 2:3]),
                  reads=[Bh_t, Bjunk], writes=[Bjunk, Bsm_t])
            P.act(lambda e, sm_t=sm_t: e.activation(out=sm_t[:, 3:4], in_=sm_t[:, 2:3], func=AF.Sqrt, scale=1.0 / 128, bias=EPS),
                  reads=[Bsm_t], writes=[Bsm_t])
            P.dve(lambda e, sm_t=sm_t: e.reciprocal(out=sm_t[:, 3:4], in_=sm_t[:, 3:4]), reads=[Bsm_t], writes=[Bsm_t])
            P.dve(lambda e, sm_t=sm_t, h_t=h_t: e.tensor_scalar(out=h_t[:], in0=h_t[:], scalar1=sm_t[:, 3:4], scalar2=None, op0=ALU.mult),
                  reads=[Bsm_t, Bh_t], writes=[Bh_t])
            bt = c.bank()
            P.pe(lambda e, bt=bt, h_t=h_t: e.transpose(c.ps[:, bt, 0:128], h_t[:], c.ident[:]), reads=[Bh_t, c.Bident], writes=[c.Bps[bt]])
            g = tt // 4
            o_t, Bo = ost[g % 2], Bost[g % 2]
            P.dve(lambda e, bt=bt, o_t=o_t, tt=tt, sl=sl, h=h: e.scalar_tensor_tensor(
                out=o_t[:, (tt % 4) * 128:(tt % 4 + 1) * 128], in0=c.ps[:, bt, 0:128], scalar=nwv(h), in1=xcb[:, sl],
                op0=ALU.mult, op1=ALU.add), reads=[c.Bps[bt], c.Bvecs, Bxc, Bo], writes=[Bo])
            if tt % 4 == 3:
                P.dma("sp", c.mixT[(4 + h) * 128:(5 + h) * 128, g * 512:(g + 1) * 512], o_t[:], reads=[Bo], writes=[c.BmixT[4 + h]])
    A.release(m)


def outproj(c, l):
    nc, P, A = c.nc, c.P, c.A
    m = A.mark()
    TT = 512
    Wo = A.alloc("Wo", [128, 8, D], BF16)
    BWo = Buf("Wo")
    P.dma("pool", Wo[:], c.mix_out[l].rearrange("(kc p) n -> p kc n", p=128), writes=[BWo])
    nb = NormBufs(c, TT, "o")
    ht = [A.alloc("oht", [128, 8, TT], F32) for _ in range(2)]
    Bht = [Buf("oht%d" % i) for i in range(2)]
    mx = [A.alloc("omx", [128, 8, TT], BF16) for _ in range(2)]
    Bmx = [Buf("omx%d" % i) for i in range(2)]
    y = A.alloc("oy", [128, 8, TT], F32)
    By = Buf("oy")
    for g in range(S // TT):
        sl = slice(g * TT, (g + 1) * TT)
        hti, Bh = ht[g % 2], Bht[g % 2]
        mxi, Bm = mx[g % 2], Bmx[g % 2]
        P.dma("sp", hti[:], hT_tile(c, g * TT, TT), reads=hT_bufs(c, g * TT, TT), writes=[Bh])
        P.dma("sp", mxi[:], c.mixT.rearrange("(kc p) t -> p kc t", p=128)[:, :, sl], reads=c.BmixT, writes=[Bm])
        for dc in range(8):
            b = c.bank()
            for kc in range(8):
                P.pe(lambda e, b=b, dc=dc, kc=kc, mxi=mxi: e.matmul(c.ps[:, b, :], Wo[:, kc, dc * 128:(dc + 1) * 128], mxi[:, kc, :],
                                                                   start=(kc == 0), stop=(kc == 7)), reads=[BWo, Bm], writes=[c.Bps[b]])
            P.act(lambda e, b=b, dc=dc: e.copy(out=y[:, dc, :], in_=c.ps[:, b, :]), reads=[c.Bps[b]], writes=[By])
        post_stage(c, nb, y[:], By, hti[:], Bh, l, 1)
        P.dma("sp", hT_tile(c, g * TT, TT), hti[:], reads=[Bh], writes=hT_bufs(c, g * TT, TT))
    A.release(m)


def pack_inputs(inputs, b):
    f = np.float32
    vecs = np.zeros((128, NV), f)
    vecs[:, CT:CT + 8] = inputs["c"][b].reshape(8, 128).T
    for l in range(NL):
        vecs[:, ADAB + l * 72:ADAB + (l + 1) * 72] = inputs["ada_b"][l].reshape(72, 128).T
        for j in range(3):
            o = (l * 3 + j) * 8
            vecs[:, PREW + o:PREW + o + 8] = inputs["pre_norm_w"][l, j].reshape(8, 128).T
            vecs[:, POSTW + o:POSTW + o + 8] = inputs["post_norm_w"][l, j].reshape(8, 128).T
        vecs[:, PSC + l * 2:PSC + l * 2 + 2] = inputs["pool_scale"][l].reshape(2, 128).T
        for tap in range(4):
            vecs[:, CW + (l * 4 + tap) * 4:CW + (l * 4 + tap) * 4 + 4] = inputs["mlstm_conv_w"][l, tap].reshape(4, 128).T
        vecs[:, CB + l * 4:CB + l * 4 + 4] = inputs["mlstm_conv_b"][l].reshape(4, 128).T
        vecs[:, NW + l * 4:NW + l * 4 + 4] = inputs["mlstm_norm_w"][l].reshape(4, 128).T
        vecs[:, SK + l * 4:SK + l * 4 + 4] = inputs["mlstm_skip"][l].reshape(4, 128).T
    gateb = np.ascontiguousarray(inputs["mlstm_gate_b"].transpose(2, 0, 1).reshape(4, 8)).astype(f)
    return vecs, gateb


_CACHE = {}


def kernel(**inputs):
    inputs = {k: np.asarray(v) for k, v in inputs.items()}
    if "nc" not in _CACHE:
        _CACHE["nc"] = build()[0]
    nc = _CACHE["nc"]
    shared = {
        "ada_w": np.ascontiguousarray(inputs["ada_w"], dtype=np.float32),
        "ffn_up": np.ascontiguousarray(inputs["ffn_up"], dtype=np.float32),
        "ffn_down": np.ascontiguousarray(inputs["ffn_down"], dtype=np.float32),
        "mix_in_w": np.ascontiguousarray(inputs["mix_in_w"], dtype=np.float32),
        "mix_out_w": np.ascontiguousarray(inputs["mix_out_w"], dtype=np.float32),
        "pool_w": np.ascontiguousarray(inputs["pool_w"], dtype=np.float32),
        "qkv_w": np.ascontiguousarray(inputs["mlstm_qkv_w"], dtype=np.float32),
    }
    in_maps = []
    for b in range(8):
        vecs, gateb = pack_inputs(inputs, b)
        d = dict(shared)
        d["x"] = np.ascontiguousarray(inputs["x"][b], dtype=np.float32)
        d["vecs"] = vecs
        d["gateb"] = gateb
        in_maps.append(d)
    res = run_bass_kernel_spmd(nc, in_maps, core_ids=list(range(8)))
    return np.stack([np.asarray(r["out"]) for r in res.results], axis=0).astype(np.float32)
```

```python
import contextlib
import numpy as np
import concourse.bass as bass
import concourse.mybir as mybir
from concourse.bass_utils import run_bass_kernel_spmd

F32 = mybir.dt.float32
BF16 = mybir.dt.bfloat16
AF = mybir.ActivationFunctionType
ALU = mybir.AluOpType

S = 4096
D = 1024
DFF = 2816
NL = 4
INW = 2056
EPS = 1e-6

CT, ADAB, PREW, POSTW, PSC, CW, CB, NW, SK, NV = 0, 8, 296, 392, 488, 496, 560, 576, 592, 608


class Buf:
    __slots__ = ("name", "last_w", "readers", "excl")

    def __init__(self, name, excl=False):
        self.name = name
        self.last_w = None
        self.readers = []
        self.excl = excl


class Op:
    __slots__ = ("idx", "eng", "fn", "dma", "deps", "signal", "count", "sem")


class _Rec:
    def __init__(self):
        self.call = None

    def __getattr__(self, name):
        def f(*a, **kw):
            assert self.call is None
            self.call = (name, a, kw)
            return None
        return f


class Prog:
    EPOCH = 30000
    NDMASEM = 24
    NSWSEM = 12

    def __init__(self, nc):
        self.nc = nc
        self.ops = []
        self.q = {"pe": [], "act": [], "dve": [], "pool": [], "sp": []}
        self._bar_idx = 0

    def op(self, eng, fn, reads=(), writes=(), dma=False):
        o = Op()
        o.idx = len(self.ops)
        o.eng = eng
        rec = _Rec()
        fn(rec)
        name, a, kw = rec.call
        o.fn = (lambda e, name=name, a=a, kw=kw: getattr(e, name)(*a, **kw))
        o.dma = dma
        o.signal = False
        o.count = None
        o.sem = None
        deps = set()
        if any(r.excl for r in reads):
            writes = list(writes) + [r for r in reads if r.excl and r not in writes]
            reads = [r for r in reads if not r.excl]
        for r in reads:
            if r.last_w is not None:
                deps.add(r.last_w)
        for w in writes:
            if w.last_w is not None:
                deps.add(w.last_w)
            for rr in w.readers:
                deps.add(rr)
        for r in reads:
            r.readers.append(o.idx)
        for w in writes:
            w.last_w = o.idx
            w.readers = []
        deps.discard(o.idx)
        o.deps = deps
        self.ops.append(o)
        self.q[eng].append(o)
        return o

    def pe(self, fn, reads=(), writes=()):
        return self.op("pe", fn, reads, writes)

    def act(self, fn, reads=(), writes=()):
        return self.op("act", fn, reads, writes)

    def dve(self, fn, reads=(), writes=()):
        return self.op("dve", fn, reads, writes)

    def pool(self, fn, reads=(), writes=()):
        return self.op("pool", fn, reads, writes)

    def dma(self, q, out, in_, reads=(), writes=(), **kw):
        return self.op(q, lambda e: e.dma_start(out=out, in_=in_, **kw), reads, writes, dma=True)

    def barrier(self):
        last = {}
        dmas = []
        for o in self.ops:
            if o.dma:
                if o.idx >= self._bar_idx:
                    dmas.append(o.idx)
            else:
                last[o.eng] = o.idx
        deps = set(last.values()) | set(dmas)
        self._bar_idx = len(self.ops)
        for eng in ["pe", "act", "dve", "pool", "sp"]:
            o = self.op(eng, lambda e: e.nop(), (), ())
            o.deps = set(deps)

    def emit(self):
        nc = self.nc
        ops = self.ops
        for cls, nds, base in (("sw", self.NSWSEM, 0), ("hw", self.NDMASEM, self.NSWSEM)):
            dma_ops = [o for o in ops if o.dma and ((o.eng == "pool") == (cls == "sw"))]
            if not dma_ops:
                continue
            nds = min(nds, len(dma_ops))
            last_on_sem = [None] * nds
            cnt_on_sem = [0] * nds
            for i, o in enumerate(dma_ops):
                s = i % nds
                if last_on_sem[s] is not None:
                    o.deps.add(last_on_sem[s])
                last_on_sem[s] = o.idx
                cnt_on_sem[s] += 16
                o.sem = ("dma", base + s)
                o.count = cnt_on_sem[s]
                o.signal = True
        for o in ops:
            nd = set()
            for d in o.deps:
                p = ops[d]
                if (not p.dma) and p.eng == "pe" and o.eng == "pe" and not o.dma:
                    continue
                nd.add(d)
            o.deps = nd
            for d in nd:
                ops[d].signal = True
        for eng, lst in self.q.items():
            ep = 0
            c = 0
            for o in lst:
                if o.dma or not o.signal:
                    continue
                if c >= self.EPOCH:
                    ep += 1
                    c = 0
                c += 1
                o.sem = (eng, ep)
                o.count = c
        semkeys = sorted(set(o.sem for o in ops if o.sem is not None))
        self.nsem = len(semkeys)
        sems = {}
        stack = contextlib.ExitStack()
        for k in semkeys:
            sems[k] = stack.enter_context(nc.semaphore("s_%s_%d" % k))
        engobj = {"pe": "tensor", "act": "scalar", "dve": "vector", "pool": "gpsimd", "sp": "sync"}
        self.nwaits = 0
        self.trace = {}
        with stack:
            with nc.Block() as block:
                def make(eng):
                    lst = self.q[eng]

                    def body(e):
                        waited = {}
                        maxep = {}
                        tr = self.trace.setdefault(eng, [])
                        for o in lst:
                            wl = []
                            need = {}
                            for d in o.deps:
                                p = ops[d]
                                k = p.sem
                                if need.get(k, 0) < p.count:
                                    need[k] = p.count
                            for k in sorted(need):
                                c = need[k]
                                if waited.get(k, 0) >= c:
                                    continue
                                if k[0] != "dma" and maxep.get(k[0], -1) > k[1]:
                                    continue
                                e.wait_ge(sems[k], c)
                                wl.append((k, c))
                                self.nwaits += 1
                                waited[k] = c
                                if k[0] != "dma":
                                    maxep[k[0]] = max(maxep.get(k[0], -1), k[1])
                            ins = o.fn(e)
                            if o.signal:
                                ins.then_inc(sems[o.sem], 16 if o.dma else 1)
                            tr.append((wl, (o.sem, 16 if o.dma else 1) if o.signal else None, o.idx))
                        fin = {}
                        for o in lst:
                            if o.dma:
                                fin[o.sem] = max(fin.get(o.sem, 0), o.count)
                        for k in sorted(fin):
                            if waited.get(k, 0) < fin[k]:
                                e.wait_ge(sems[k], fin[k])
                    return body
                for eng in ["sp", "act", "pool", "dve", "pe"]:
                    if self.q[eng]:
                        getattr(block, engobj[eng])(make(eng))


def check_protocol(trace):
    sem = {}
    pc = {e: 0 for e in trace}
    progress = True
    while progress:
        progress = False
        for e, tr in trace.items():
            while pc[e] < len(tr):
                wl, sig, idx = tr[pc[e]]
                if all(sem.get(k, 0) >= c for k, c in wl):
                    if sig is not None:
                        sem[sig[0]] = sem.get(sig[0], 0) + sig[1]
                    pc[e] += 1
                    progress = True
                else:
                    break
    stuck = {e: (pc[e], len(tr)) for e, tr in trace.items() if pc[e] < len(tr)}
    return stuck, sem


class Arena:
    LO = 16512
    HI = 229344

    def __init__(self, nc):
        self.nc = nc
        self.top = self.LO
        self.n = 0
        self.peak = 0

    def alloc(self, name, shape, dt):
        esz = 4 if dt == F32 else 2
        nbytes = esz
        for s in shape[1:]:
            nbytes *= s
        off = (self.top + 63) // 64 * 64
        if off + nbytes > self.HI:
            raise RuntimeError("SBUF arena overflow allocating %s %s: off=%d need=%d" % (name, shape, off, nbytes))
        self.top = off + nbytes
        self.peak = max(self.peak, self.top)
        self.n += 1
        return self.nc.alloc_sbuf_tensor_at("%s_%d" % (name, self.n), list(shape), dt, offset=off)

    def mark(self):
        return self.top

    def release(self, m):
        self.top = m


class Ctx:
    pass


def build(n_sub=12, debug=(), parts=None, small_ffn=False):
    nc = bass.Bass("TRN2", target_bir_lowering=False)
    c = Ctx()
    c.nc = nc
    c.P = P = Prog(nc)
    c.A = A = Arena(nc)
    c.debug = debug
    c.dbg_out = {}
    c.parts = parts

    def din(name, shape, dt=F32):
        return nc.dram_tensor(name, list(shape), dt, kind="ExternalInput").ap()

    c.x = din("x", [S, D])
    c.vecs_d = din("vecs", [128, NV])
    c.gateb_d = din("gateb", [4, 8])
    c.ada_w = din("ada_w", [NL, D, 9 * D])
    if small_ffn:
        c.ffn_up = din("ffn_up", [1, 1, 128, 128])
        c.ffn_down = din("ffn_down", [1, 1, 128, 128])
    else:
        c.ffn_up = din("ffn_up", [NL, 2, D, 2 * DFF])
        c.ffn_down = din("ffn_down", [NL, 2, DFF, D])
    c.mix_in = din("mix_in_w", [NL, D, INW])
    c.mix_out = din("mix_out_w", [NL, D, D])
    c.pool_w = din("pool_w", [NL, 4, 64, 64])
    c.qkv_w = din("qkv_w", [NL, 3, 4, 128, 128])
    c.out = nc.dram_tensor("out", [S, D], F32, kind="ExternalOutput").ap()
    c.hT = nc.dram_tensor("hT", [D, S], F32, kind="Internal").ap()
    c.mixT = nc.dram_tensor("mixT", [D, S], BF16, kind="Internal").ap()
    c.BhT = [Buf("hT%d" % i) for i in range(S // 256)]
    c.BmixT = [Buf("mixT%d" % i) for i in range(8)]
    c.ps = nc.alloc_psum_tensor("ps", [128, 8, 512], F32)
    c.Bps = [Buf("ps%d" % i, excl=True) for i in range(8)]
    c.rr = 0

    def bank():
        i = c.rr % 8
        c.rr += 1
        return i
    c.bank = bank

    def dbg(name, shape, dt=F32):
        t = nc.dram_tensor("dbg_" + name, list(shape), dt, kind="ExternalOutput").ap()
        c.dbg_out[name] = t
        return t
    c.dbg = dbg

    prologue(c)
    P.barrier()
    t_in(c)
    nsub = 0
    for l in range(NL):
        for j in range(3):
            if nsub >= n_sub:
                break
            P.barrier()
            if j == 1:
                mixer_phase(c, l)
            elif not small_ffn:
                ffn_phase(c, l, j)
            nsub += 1
    P.barrier()
    t_out(c)
    P.emit()
    stuck, semv = check_protocol(P.trace)
    if stuck:
        raise RuntimeError("protocol deadlock: %s" % stuck)
    c.maxsem = max(semv.values())
    c.stats = dict(nops=len(P.ops), nsem=P.nsem, nwaits=P.nwaits, peak=A.peak,
                   per_eng={k: len(v) for k, v in P.q.items()})
    return nc, c


def hT_tile(c, t0, tt):
    return c.hT.rearrange("(dc p) t -> p dc t", p=128)[:, :, t0:t0 + tt]


def hT_bufs(c, t0, tt):
    return c.BhT[t0 // 256:(t0 + tt + 255) // 256]


def prologue(c):
    nc, P, A = c.nc, c.P, c.A
    c.vecs = A.alloc("vecs", [128, NV], F32)
    c.Bvecs = Buf("vecs")
    P.dma("sp", c.vecs[:], c.vecs_d, writes=[c.Bvecs])
    c.gateb = A.alloc("gateb", [4, 8], F32)
    c.Bgateb = Buf("gateb")
    P.dma("sp", c.gateb[:], c.gateb_d, writes=[c.Bgateb])
    c.ones_bf = A.alloc("ones", [128, 128], BF16)
    c.Bones = Buf("ones")
    P.dve(lambda e: e.memset(c.ones_bf[:], 1.0), writes=[c.Bones])
    c.ident = A.alloc("ident", [128, 128], F32)
    c.Bident = Buf("ident")
    P.pool(lambda e: e.memset(c.ident[:], 1.0), writes=[c.Bident])
    P.pool(lambda e: e.affine_select(out=c.ident[:], in_=c.ident[:], pattern=[[-1, 128]], compare_op=ALU.is_equal,
                                     fill=0.0, base=0, channel_multiplier=1), reads=[c.Bident], writes=[c.Bident])
    c.selh = A.alloc("selh", [4, 4, 128], F32)
    c.Bselh = Buf("selh")
    P.pool(lambda e: e.memset(c.selh[:], 1.0), writes=[c.Bselh])
    P.pool(lambda e: e.affine_select(out=c.selh[:], in_=c.selh[:], pattern=[[-1, 4], [0, 128]], compare_op=ALU.is_equal,
                                     fill=0.0, base=0, channel_multiplier=1), reads=[c.Bselh], writes=[c.Bselh])
    mk = A.alloc("mk32", [128, 512], F32)
    Bmk = Buf("mk32")
    c.amask = A.alloc("amask", [128, 512], BF16)
    c.Bamask = Buf("amask")
    P.pool(lambda e: e.memset(mk[:], 1.0), writes=[Bmk])
    for i in range(4):
        sl = mk[:, i * 128:(i + 1) * 128]
        if i % 2 == 0:
            P.pool(lambda e, sl=sl: e.affine_select(out=sl, in_=sl, pattern=[[-1, 128]], compare_op=ALU.is_ge, fill=0.0,
                                                    base=0, channel_multiplier=1), reads=[Bmk], writes=[Bmk])
        else:
            P.pool(lambda e, sl=sl: e.affine_select(out=sl, in_=sl, pattern=[[1, 128]], compare_op=ALU.is_ge, fill=0.0,
                                                    base=0, channel_multiplier=-1), reads=[Bmk], writes=[Bmk])
    P.dve(lambda e: e.tensor_copy(out=c.amask[:], in_=mk[:]), reads=[Bmk], writes=[c.Bamask])
    mk2 = A.alloc("mk2", [128, 128], F32)
    Bmk2 = Buf("mk2")
    c.lmask = A.alloc("lmask", [128, 128], BF16)
    c.Blmask = Buf("lmask")
    P.pool(lambda e: e.memset(mk2[:], 1.0), writes=[Bmk2])
    P.pool(lambda e: e.affine_select(out=mk2[:], in_=mk2[:], pattern=[[1, 128]], compare_op=ALU.is_ge, fill=0.0,
                                     base=0, channel_multiplier=-1), reads=[Bmk2], writes=[Bmk2])
    P.pool(lambda e: e.memset(mk2[0:64, 64:128], 0.0), reads=[Bmk2], writes=[Bmk2])
    P.dve(lambda e: e.tensor_copy(out=c.lmask[:], in_=mk2[:]), reads=[Bmk2], writes=[c.Blmask])
    c.invw = A.alloc("invw", [128, 2], F32)
    c.invcnt = A.alloc("invcnt", [128, 2, 16], F32)
    c.Bpoolc = Buf("poolc")
    wins = (2, 4, 8, 16)
    first = True
    for pc in range(2):
        for half in range(2):
            w = wins[pc * 2 + half]
            pr = slice(half * 64, half * 64 + 64)
            P.pool(lambda e, pr=pr, pc=pc, w=w: e.memset(c.invw[pr, pc:pc + 1], 1.0 / w), reads=[c.Bpoolc], writes=[c.Bpoolc])
            for t in range(16):
                P.pool(lambda e, pr=pr, pc=pc, w=w, t=t: e.memset(c.invcnt[pr, pc, t:t + 1], 1.0 / min(t + 1, w)),
                       reads=[c.Bpoolc], writes=[c.Bpoolc])
    c.cact = A.alloc("cact", [128, 8], BF16)
    c.Bcact = Buf("cact")
    P.act(lambda e: e.activation(out=c.cact[:], in_=c.vecs[:, CT:CT + 8], func=AF.Silu), reads=[c.Bvecs], writes=[c.Bcact])
    c.modT = A.alloc("modT", [128, NL * 72], F32)
    c.Bmod = Buf("modT")
    m = A.mark()
    NPC = 8
    PW = 9 * D // NPC
    pieces = [A.alloc("adap", [128, 8, PW], BF16) for _ in range(2)]
    Bpieces = [Buf("adap0"), Buf("adap1")]
    mb = c.bank()
    k = 0
    for l in range(NL):
        src = c.ada_w[l].rearrange("(kc p) n -> p kc n", p=128)
        for pc in range(NPC):
            pt, Bp = pieces[k % 2], Bpieces[k % 2]
            k += 1
            P.dma("pool", pt[:], src[:, :, pc * PW:(pc + 1) * PW], writes=[Bp])
            for ocl in range(PW // 128):
                oc = pc * (PW // 128) + ocl
                col = l * 72 + oc
                for kc in range(8):
                    P.pe(lambda e, pt=pt, kc=kc, ocl=ocl, col=col: e.matmul(
                        c.ps[:, mb, col:col + 1], pt[:, kc, ocl * 128:(ocl + 1) * 128], c.cact[:, kc:kc + 1],
                        start=(kc == 0), stop=(kc == 7)), reads=[Bp, c.Bcact], writes=[c.Bps[mb]])
    P.dve(lambda e: e.tensor_tensor(out=c.modT[:], in0=c.ps[:, mb, 0:NL * 72], in1=c.vecs[:, ADAB:ADAB + NL * 72], op=ALU.add),
          reads=[c.Bps[mb], c.Bvecs], writes=[c.Bmod])
    A.release(m)
    c.Avec = A.alloc("Avec", [128, NL * 3 * 8], F32)
    c.Gvec = A.alloc("Gvec", [128, NL * 3 * 8], F32)
    c.Bder = Buf("der")
    for l in range(NL):
        for j in range(3):
            o = (l * 3 + j) * 8
            sc = c.modT[:, l * 72 + (j * 3 + 1) * 8: l * 72 + (j * 3 + 1) * 8 + 8]
            gt = c.modT[:, l * 72 + (j * 3 + 2) * 8: l * 72 + (j * 3 + 2) * 8 + 8]
            wgt = 1.0 if j == 1 else 0.5
            P.dve(lambda e, o=o, sc=sc: e.scalar_tensor_tensor(out=c.Avec[:, o:o + 8], in0=sc, scalar=1.0,
                                                               in1=c.vecs[:, PREW + o:PREW + o + 8], op0=ALU.add, op1=ALU.mult),
                  reads=[c.Bmod, c.Bvecs], writes=[c.Bder])
            P.dve(lambda e, o=o, gt=gt, wgt=wgt: e.scalar_tensor_tensor(out=c.Gvec[:, o:o + 8], in0=gt, scalar=wgt,
                                                                        in1=c.vecs[:, POSTW + o:POSTW + o + 8], op0=ALU.mult, op1=ALU.mult),
                  reads=[c.Bmod, c.Bvecs], writes=[c.Bder])
    if "mod" in c.debug:
        d = c.dbg("mod", [128, NL * 72])
        P.dma("sp", d, c.modT[:], reads=[c.Bmod])


def shift_ap(c, l, j, dc):
    o = l * 72 + (j * 3 + 0) * 8 + dc
    return c.modT[:, o:o + 1]


def A_ap(c, l, j, dc):
    o = (l * 3 + j) * 8 + dc
    return c.Avec[:, o:o + 1]


def G_ap(c, l, j, dc):
    o = (l * 3 + j) * 8 + dc
    return c.Gvec[:, o:o + 1]


def t_in(c):
    nc, P, A = c.nc, c.P, c.A
    m = A.mark()
    NX = 8
    xt = [A.alloc("xt", [128, D], F32) for _ in range(NX)]
    Bxt = [Buf("xt%d" % i) for i in range(NX)]
    hst = [A.alloc("hst", [128, 8, 512], F32) for _ in range(2)]
    Bhst = [Buf("hst%d" % i) for i in range(2)]
    ev = 0
    for g in range(S // 512):
        hs, Bh = hst[g % 2], Bhst[g % 2]
        for half in range(2):
            banks = [c.bank() for _ in range(4)]
            for tt in range(4):
                xi = (g * 4 + tt) % NX
                if half == 0:
                    r0 = g * 512 + tt * 128
                    P.dma("sp", xt[xi][:], c.x[r0:r0 + 128, :], writes=[Bxt[xi]])
                for d in range(4):
                    dc = half * 4 + d
                    P.pe(lambda e, b=banks[d], tt=tt, xi=xi, dc=dc: e.transpose(
                        c.ps[:, b, tt * 128:(tt + 1) * 128], xt[xi][:, dc * 128:(dc + 1) * 128], c.ident[:]),
                        reads=[Bxt[xi], c.Bident], writes=[c.Bps[banks[d]]])
            for d in range(4):
                dc = half * 4 + d
                b = banks[d]
                if ev % 2 == 0:
                    P.act(lambda e, b=b, dc=dc, hs=hs: e.copy(out=hs[:, dc, :], in_=c.ps[:, b, :]), reads=[c.Bps[b]], writes=[Bh])
                else:
                    P.dve(lambda e, b=b, dc=dc, hs=hs: e.tensor_copy(out=hs[:, dc, :], in_=c.ps[:, b, :]), reads=[c.Bps[b]], writes=[Bh])
                ev += 1
        P.dma("sp", hT_tile(c, g * 512, 512), hs[:], reads=[Bh], writes=hT_bufs(c, g * 512, 512))
    A.release(m)


def t_out(c):
    nc, P, A = c.nc, c.P, c.A
    m = A.mark()
    hst = [A.alloc("hst", [128, 8, 512], F32) for _ in range(2)]
    Bhst = [Buf("ohst%d" % i) for i in range(2)]
    NO = 4
    ot = [A.alloc("ot", [128, D], F32) for _ in range(NO)]
    Bot = [Buf("ot%d" % i) for i in range(NO)]
    k = 0
    for g in range(S // 512):
        hs, Bh = hst[g % 2], Bhst[g % 2]
        P.dma("sp", hs[:], hT_tile(c, g * 512, 512), reads=hT_bufs(c, g * 512, 512), writes=[Bh])
        for tt in range(4):
            o, Bo = ot[k % NO], Bot[k % NO]
            k += 1
            for half in range(2):
                b = c.bank()
                for d in range(4):
                    dc = half * 4 + d
                    P.pe(lambda e, b=b, d=d, dc=dc, hs=hs, tt=tt: e.transpose(
                        c.ps[:, b, d * 128:(d + 1) * 128], hs[:, dc, tt * 128:(tt + 1) * 128], c.ident[:]),
                        reads=[Bh, c.Bident], writes=[c.Bps[b]])
                if half == 0:
                    P.act(lambda e, b=b, o=o: e.copy(out=o[:, 0:512], in_=c.ps[:, b, :]), reads=[c.Bps[b]], writes=[Bo])
                else:
                    P.dve(lambda e, b=b, o=o: e.tensor_copy(out=o[:, 512:1024], in_=c.ps[:, b, :]), reads=[c.Bps[b]], writes=[Bo])
            r0 = g * 512 + tt * 128
            P.dma("sp", c.out[r0:r0 + 128, :], o[:], reads=[Bo])
    A.release(m)


class NormBufs:
    def __init__(self, c, TT, name, ntmp=1):
        A = c.A
        self.TT = TT
        self.sq = [A.alloc(name + "sq", [128, 8, TT], BF16) for _ in range(2)]
        self.Bsq = [Buf(name + "sq%d" % i) for i in range(2)]
        self.rs = [A.alloc(name + "rs", [128, TT], F32) for _ in range(2)]
        self.Brs = [Buf(name + "rs%d" % i) for i in range(2)]
        self.tmps = [A.alloc(name + "tmp", [128, 8, TT], F32) for _ in range(ntmp)]
        self.Btmps = [Buf(name + "tmp%d" % i) for i in range(ntmp)]
        self.tk = 0
        self.k = 0

    def next_tmp(self):
        i = self.tk % len(self.tmps)
        self.tk += 1
        return self.tmps[i], self.Btmps[i]


def sumsq_rstd(c, nb, src, Bsrc):
    P = c.P
    TT = nb.TT
    i = nb.k % 2
    nb.k += 1
    sq, Bsq, rs, Brs = nb.sq[i], nb.Bsq[i], nb.rs[i], nb.Brs[i]
    P.pool(lambda e: e.tensor_tensor(out=sq[:], in0=src, in1=src, op=ALU.mult), reads=[Bsrc], writes=[Bsq])
    b = c.bank()
    for dc in range(8):
        P.pe(lambda e, dc=dc: e.matmul(c.ps[:, b, 0:TT], c.ones_bf[:], sq[:, dc, :], start=(dc == 0), stop=(dc == 7)),
             reads=[Bsq, c.Bones], writes=[c.Bps[b]])
    P.act(lambda e: e.activation(out=rs[:], in_=c.ps[:, b, 0:TT], func=AF.Sqrt, scale=1.0 / D, bias=EPS),
          reads=[c.Bps[b]], writes=[Brs])
    P.dve(lambda e: e.reciprocal(out=rs[:], in_=rs[:]), reads=[Brs], writes=[Brs])
    return rs, Brs


def pre_stage(c, nb, ht, Bht, l, j, uT_dst, BuT):
    P = c.P
    TT = nb.TT
    rs, Brs = sumsq_rstd(c, nb, ht, Bht)
    tmp, Btmp = nb.next_tmp()
    P.dve(lambda e: e.tensor_tensor(out=tmp[:], in0=ht, in1=rs[:, :].unsqueeze(1).broadcast_to([128, 8, TT]), op=ALU.mult),
          reads=[Bht, Brs], writes=[Btmp])
    for dc in range(8):
        P.act(lambda e, dc=dc: e.activation(out=uT_dst[:, dc, :], in_=tmp[:, dc, :], func=AF.Identity,
                                            scale=A_ap(c, l, j, dc), bias=shift_ap(c, l, j, dc)),
              reads=[Btmp, c.Bder, c.Bmod], writes=[BuT])


def post_stage(c, nb, y, By, ht, Bht, l, j):
    P = c.P
    TT = nb.TT
    rs, Brs = sumsq_rstd(c, nb, y, By)
    tmp, Btmp = nb.next_tmp()
    P.dve(lambda e: e.tensor_tensor(out=tmp[:], in0=y, in1=rs[:, :].unsqueeze(1).broadcast_to([128, 8, TT]), op=ALU.mult),
          reads=[By, Brs], writes=[Btmp])
    for dc in range(8):
        P.dve(lambda e, dc=dc: e.scalar_tensor_tensor(out=ht[:, dc, :], in0=tmp[:, dc, :], scalar=G_ap(c, l, j, dc),
                                                      in1=ht[:, dc, :], op0=ALU.mult, op1=ALU.add),
              reads=[Btmp, c.Bder, Bht], writes=[Bht])


def ffn_phase(c, l, j):
    nc, P, A = c.nc, c.P, c.A
    fi = 0 if j == 0 else 1
    m = A.mark()
    TT = 256
    NT = S // TT
    NKF = DFF // 128
    Wup = A.alloc("Wup", [128, 8, 2 * DFF], BF16)
    BWup = [Buf("Wup%d" % k) for k in range(8)]
    Wdn = A.alloc("Wdn", [128, NKF, D], BF16)
    BWdn = [Buf("Wdn%d" % k) for k in range(2)]
    for kc in range(8):
        P.dma("pool", Wup[:, kc, :], c.ffn_up[l, fi, kc * 128:(kc + 1) * 128, :], writes=[BWup[kc]])
    dsrc = c.ffn_down[l, fi].rearrange("(kc p) n -> p kc n", p=128)
    for pc in range(2):
        P.dma("pool", Wdn[:, pc * 11:(pc + 1) * 11, :], dsrc[:, pc * 11:(pc + 1) * 11, :], writes=[BWdn[pc]])
    nb = NormBufs(c, TT, "f")
    ht = [A.alloc("ht", [128, 8, TT], F32) for _ in range(2)]
    Bht = [Buf("ht%d" % i) for i in range(2)]
    uT = [A.alloc("uT", [128, 8, TT], BF16) for _ in range(2)]
    BuT = [Buf("uT%d" % i) for i in range(2)]
    sg = [A.alloc("sg", [128, TT], F32) for _ in range(3)]
    Bsg = [Buf("sg%d" % i) for i in range(3)]
    hid = A.alloc("hid", [128, NKF, TT], BF16)
    Bhid = [Buf("hid%d" % k) for k in range(NKF)]
    y = A.alloc("y", [128, 8, TT], F32)
    By = Buf("y")

    def load(i):
        P.dma("sp", ht[i % 2][:], hT_tile(c, i * TT, TT), reads=hT_bufs(c, i * TT, TT), writes=[Bht[i % 2]])

    def pre(i):
        pre_stage(c, nb, ht[i % 2][:], Bht[i % 2], l, j, uT[i % 2], BuT[i % 2])

    sgk = [0]

    def up(i):
        u = uT[i % 2]
        for jj in range(NKF):
            b = c.bank()
            for half in range(2):
                col0 = half * DFF + jj * 128
                for kc in range(8):
                    P.pe(lambda e, b=b, half=half, col0=col0, kc=kc, u=u: e.matmul(
                        c.ps[:, b, half * TT:(half + 1) * TT], Wup[:, kc, col0:col0 + 128], u[:, kc, :],
                        start=(kc == 0), stop=(kc == 7)), reads=[BWup[kc], BuT[i % 2]], writes=[c.Bps[b]])
            si = sgk[0] % 3
            sgk[0] += 1
            P.act(lambda e, b=b, si=si: e.activation(out=sg[si][:], in_=c.ps[:, b, 0:TT], func=AF.Silu),
                  reads=[c.Bps[b]], writes=[Bsg[si]])
            P.dve(lambda e, b=b, si=si, jj=jj: e.tensor_tensor(out=hid[:, jj, :], in0=sg[si][:], in1=c.ps[:, b, TT:2 * TT], op=ALU.mult),
                  reads=[c.Bps[b], Bsg[si]], writes=[Bhid[jj]])

    def down(i):
        for dc in range(8):
            b = c.bank()
            for kc in range(NKF):
                P.pe(lambda e, b=b, dc=dc, kc=kc: e.matmul(
                    c.ps[:, b, 0:TT], Wdn[:, kc, dc * 128:(dc + 1) * 128], hid[:, kc, :],
                    start=(kc == 0), stop=(kc == NKF - 1)), reads=[BWdn[kc // 11], Bhid[kc]], writes=[c.Bps[b]])
            P.act(lambda e, b=b, dc=dc: e.copy(out=y[:, dc, :], in_=c.ps[:, b, 0:TT]), reads=[c.Bps[b]], writes=[By])

    def post(i):
        post_stage(c, nb, y[:], By, ht[i % 2][:], Bht[i % 2], l, j)
        P.dma("sp", hT_tile(c, i * TT, TT), ht[i % 2][:], reads=[Bht[i % 2]], writes=hT_bufs(c, i * TT, TT))

    load(0)
    pre(0)
    for i in range(NT):
        if i + 1 < NT:
            load(i + 1)
        up(i)
        if i + 1 < NT:
            pre(i + 1)
        down(i)
        post(i)
    A.release(m)


def mixer_phase(c, l):
    nc, P, A = c.nc, c.P, c.A
    m = A.mark()
    uT = A.alloc("uTall", [128, 8, S], BF16)
    BuT = [Buf("uTall%d" % g) for g in range(8)]
    Win = A.alloc("Win", [128, 8, INW], BF16)
    BWin = Buf("Win")
    P.dma("pool", Win[:], c.mix_in[l].rearrange("(kc p) n -> p kc n", p=128), writes=[BWin])
    m0 = A.mark()
    TT = 512
    nb = NormBufs(c, TT, "m", ntmp=2)
    ht = [A.alloc("mht", [128, 8, TT], F32) for _ in range(2)]
    Bht = [Buf("mht%d" % i) for i in range(2)]
    for g in range(S // TT):
        P.dma("sp", ht[g % 2][:], hT_tile(c, g * TT, TT), reads=hT_bufs(c, g * TT, TT), writes=[Bht[g % 2]])
        pre_stage(c, nb, ht[g % 2][:], Bht[g % 2], l, 1, uT[:, :, g * TT:(g + 1) * TT], BuT[g])
    A.release(m0)
    P.barrier()
    if "uT" in c.debug and l == 0:
        d = c.dbg("uT", [128, 8, S], BF16)
        P.dma("sp", d, uT[:], reads=BuT)
    mctx = Ctx()
    mctx.uT, mctx.BuT, mctx.Win, mctx.BWin = uT, BuT, Win, BWin
    pp = c.parts
    if pp is None or "att" in pp:
        attention(c, l, mctx)
        P.barrier()
    if pp is None or "pool" in pp:
        pool_mixer(c, l, mctx)
        P.barrier()
    if pp is None or "mlstm" in pp or "gates" in pp:
        mlstm(c, l, mctx)
        P.barrier()
    A.release(m)
    if pp is None or "outproj" in pp:
        outproj(c, l)


def inproj_fm(c, mctx, col0, ncols, g, b, boff=0, prow=None):
    P = c.P
    for kc in range(8):
        P.pe(lambda e, kc=kc: e.matmul(c.ps[0:ncols, b, boff:boff + 512], mctx.Win[:, kc, col0:col0 + ncols],
                                       mctx.uT[:, kc, g * 512:(g + 1) * 512], start=(kc == 0), stop=(kc == 7)),
             reads=[mctx.BWin, mctx.BuT[g]], writes=[c.Bps[b]])


def attention(c, l, mctx):
    nc, P, A = c.nc, c.P, c.A
    m = A.mark()
    uT, BuT, Win, BWin = mctx.uT, mctx.BuT, mctx.Win, mctx.BWin
    qT = A.alloc("qT", [128, S], BF16)
    kT = A.alloc("kT", [128, S], BF16)
    Bq, Bk = Buf("qT"), Buf("kT")
    ve = A.alloc("ve", [128, 32, 68], BF16)
    vo = A.alloc("vo", [128, 32, 128], BF16)
    Bve, Bvo = Buf("ve"), Buf("vo")
    acc_e = A.alloc("acce", [65, S], F32)
    acc_o = A.alloc("acco", [128, S], F32)
    Bacc = [Buf("acc%d" % g) for g in range(8)]
    pT = [A.alloc("pT", [128, 512], BF16) for _ in range(4)]
    BpT = [Buf("pT%d" % i) for i in range(4)]
    rec = A.alloc("rec", [128, 512], F32)
    Brec = Buf("rec")
    ost = [A.alloc("ost", [128, 512], BF16) for _ in range(2)]
    Bost = [Buf("ost%d" % i) for i in range(2)]
    sele = A.alloc("sele", [65, 128], F32)
    selo = A.alloc("selo", [128, 128], F32)
    Bsel = Buf("sel")
    P.pool(lambda e: e.memset(sele[:], 0.0), writes=[Bsel])
    P.pool(lambda e: e.memset(selo[:], 0.0), reads=[Bsel], writes=[Bsel])
    P.pool(lambda e: e.memset(sele[64:65, 0:64], 1.0), reads=[Bsel], writes=[Bsel])
    P.pool(lambda e: e.memset(selo[0:1, 64:128], 1.0), reads=[Bsel], writes=[Bsel])
    pk = [0]
    ok = [0]
    branches = ((128, 1), (512, 4), (2048, 16))
    for pair in range(2):
        for g in range(8):
            b = c.bank()
            inproj_fm(c, mctx, pair * 128, 128, g, b)
            P.act(lambda e, b=b, g=g: e.copy(out=qT[:, g * 512:(g + 1) * 512], in_=c.ps[:, b, :]), reads=[c.Bps[b]], writes=[Bq])
            b = c.bank()
            inproj_fm(c, mctx, 256 + pair * 128, 128, g, b)
            P.dve(lambda e, b=b, g=g: e.tensor_copy(out=kT[:, g * 512:(g + 1) * 512], in_=c.ps[:, b, :]), reads=[c.Bps[b]], writes=[Bk])
        P.pool(lambda e: e.memset(ve[:, :, 64:65], 1.0), reads=[Bve], writes=[Bve])
        P.pool(lambda e: e.memset(vo[:, :, 0:64], 0.0), reads=[Bvo], writes=[Bvo])
        P.pool(lambda e: e.memset(vo[:, :, 0:1], 1.0), reads=[Bvo], writes=[Bvo])
        for bi, (win, dil) in enumerate(branches):
            L = S // dil
            nbk = L // 128

            def tok(r, n, dil=dil):
                start = r + dil * 128 * n
                return slice(start, start + dil * 127 + 1, dil)
            blocks = [(r, n) for n in range(nbk) for r in range(dil)]
            if dil == 1:
                blocks = [(0, n) for n in range(nbk)]
            for q4 in range(8):
                b = c.bank()
                for i4 in range(4):
                    bl = q4 * 4 + i4
                    r, n = blocks[bl]
                    for kc in range(8):
                        P.pe(lambda e, b=b, i4=i4, kc=kc, ts=tok(r, n): e.matmul(
                            c.ps[:, b, i4 * 128:(i4 + 1) * 128], uT[:, kc, ts], Win[:, kc, 512 + pair * 128: 512 + pair * 128 + 128],
                            start=(kc == 0), stop=(kc == 7)), reads=BuT + [BWin], writes=[c.Bps[b]])
                psv = c.ps[:, b, :].rearrange("p (i f) -> p i f", f=128)
                P.act(lambda e, psv=psv, q4=q4: e.copy(out=ve[:, q4 * 4:(q4 + 1) * 4, 0:64], in_=psv[:, :, 0:64]),
                      reads=[c.Bps[b]], writes=[Bve])
                P.dve(lambda e, psv=psv, q4=q4: e.tensor_copy(out=vo[:, q4 * 4:(q4 + 1) * 4, 64:128], in_=psv[:, :, 64:128]),
                      reads=[c.Bps[b]], writes=[Bvo])
            blk_index = {rn: i for i, rn in enumerate(blocks)}
            for hh in range(2):
                pr = slice(hh * 64, hh * 64 + 64)
                vx, Bvx, M = (ve, Bve, 65) if hh == 0 else (vo, Bvo, 128)
                vcol = slice(0, M)
                acc = acc_e if hh == 0 else acc_o
                for q4 in range(8):
                    bo = c.bank()
                    for i2 in range(2):
                        bs = c.bank()
                        blist = [blocks[q4 * 4 + i2 * 2 + t] for t in range(2)]
                        for t, (r, n) in enumerate(blist):
                            qs = tok(r, n)
                            if True:
                                P.pe(lambda e, bs=bs, t=t, ks=tok(r, max(n - 1, 0)), qs=qs: e.matmul(
                                    c.ps[:, bs, (2 * t) * 128:(2 * t + 1) * 128], kT[pr, ks], qT[pr, qs], start=True, stop=True),
                                    reads=[Bq, Bk], writes=[c.Bps[bs]])
                            P.pe(lambda e, bs=bs, t=t, qs=qs: e.matmul(
                                c.ps[:, bs, (2 * t + 1) * 128:(2 * t + 2) * 128], kT[pr, qs], qT[pr, qs], start=True, stop=True),
                                reads=[Bq, Bk], writes=[c.Bps[bs]])
                        pi = pk[0] % 4
                        pk[0] += 1
                        p_t, Bp = pT[pi], BpT[pi]
                        P.act(lambda e, bs=bs, p_t=p_t: e.activation(out=p_t[:], in_=c.ps[:, bs, :], func=AF.Exp, scale=0.125),
                              reads=[c.Bps[bs]], writes=[Bp])
                        P.dve(lambda e, p_t=p_t: e.tensor_tensor(out=p_t[:], in0=p_t[:], in1=c.amask[:], op=ALU.mult),
                              reads=[Bp, c.Bamask], writes=[Bp])
                        for t, (r, n) in enumerate(blist):
                            i4 = i2 * 2 + t
                            bl = blk_index[(r, n)]
                            if n >= 1:
                                blp = blk_index[(r, n - 1)]
                                P.pe(lambda e, bo=bo, i4=i4, blp=blp, t=t, p_t=p_t, vx=vx, M=M: e.matmul(
                                    c.ps[0:M, bo, i4 * 128:(i4 + 1) * 128], vx[:, blp, 0:M], p_t[:, (2 * t) * 128:(2 * t + 1) * 128],
                                    start=True, stop=False), reads=[Bvx, Bp], writes=[c.Bps[bo]])
                            P.pe(lambda e, bo=bo, i4=i4, bl=bl, t=t, p_t=p_t, vx=vx, M=M, n=n: e.matmul(
                                c.ps[0:M, bo, i4 * 128:(i4 + 1) * 128], vx[:, bl, 0:M], p_t[:, (2 * t + 1) * 128:(2 * t + 2) * 128],
                                start=(n == 0), stop=True), reads=[Bvx, Bp], writes=[c.Bps[bo]])
                    r0, n0 = blocks[q4 * 4]
                    if dil == 1:
                        t0 = n0 * 128
                        av = acc[0:M, t0:t0 + 512]
                        pv = c.ps[0:M, bo, :]
                        gset = [Bacc[t0 // 512]]
                    elif dil == 4:
                        t0 = 512 * n0
                        av = acc[0:M, t0:t0 + 512].rearrange("p (q r) -> p r q", r=4)
                        pv = c.ps[0:M, bo, :].rearrange("p (r q) -> p r q", r=4)
                        gset = [Bacc[t0 // 512]]
                    else:
                        t0 = 2048 * n0
                        av = acc[0:M, t0:t0 + 2048].rearrange("p (q r) -> p r q", r=16)[:, r0:r0 + 4, :]
                        pv = c.ps[0:M, bo, :].rearrange("p (r q) -> p r q", r=4)
                        gset = Bacc[t0 // 512: t0 // 512 + 4]
                    if bi == 0:
                        P.act(lambda e, av=av, pv=pv: e.copy(out=av, in_=pv), reads=[c.Bps[bo]], writes=gset)
                    else:
                        P.dve(lambda e, av=av, pv=pv: e.tensor_tensor(out=av, in0=av, in1=pv, op=ALU.add),
                              reads=[c.Bps[bo]] + gset, writes=gset)
        for g in range(8):
            b = c.bank()
            sl = slice(g * 512, (g + 1) * 512)
            P.pe(lambda e, b=b, sl=sl: e.matmul(c.ps[:, b, :], sele[:], acc_e[:, sl], start=True, stop=False),
                 reads=[Bsel, Bacc[g]], writes=[c.Bps[b]])
            P.pe(lambda e, b=b, sl=sl: e.matmul(c.ps[:, b, :], selo[:], acc_o[:, sl], start=False, stop=True),
                 reads=[Bsel, Bacc[g]], writes=[c.Bps[b]])
            P.dve(lambda e, b=b: e.reciprocal(out=rec[:], in_=c.ps[:, b, :]), reads=[c.Bps[b]], writes=[Brec])
            oi = ok[0] % 2
            ok[0] += 1
            o_t, Bo = ost[oi], Bost[oi]
            P.dve(lambda e, sl=sl, o_t=o_t: e.tensor_tensor(out=o_t[0:64, :], in0=acc_e[0:64, sl], in1=rec[0:64, :], op=ALU.mult),
                  reads=[Bacc[g], Brec], writes=[Bo])
            P.pool(lambda e, sl=sl, o_t=o_t: e.tensor_tensor(out=o_t[64:128, :], in0=acc_o[64:128, sl], in1=rec[64:128, :], op=ALU.mult),
                   reads=[Bacc[g], Brec, Bo], writes=[Bo])
            P.dma("sp", c.mixT[pair * 128:(pair + 1) * 128, sl], o_t[:], reads=[Bo], writes=[c.BmixT[pair]])
    A.release(m)


def pool_mixer(c, l, mctx):
    nc, P, A = c.nc, c.P, c.A
    m = A.mark()
    uT, BuT, Win, BWin = mctx.uT, mctx.BuT, mctx.Win, mctx.BWin
    p32 = A.alloc("p32", [128, S], F32)
    bA = A.alloc("pbA", [128, S], F32)
    bB = A.alloc("pbB", [128, S], F32)
    dT = A.alloc("pdT", [128, S], BF16)
    Bp, BA_, BB_, Bd = Buf("p32"), Buf("pbA"), Buf("pbB"), Buf("pdT")
    PW = A.alloc("PWbd", [128, 128], BF16)
    BPW = Buf("PWbd")
    ost = [A.alloc("post", [128, 512], BF16) for _ in range(2)]
    Bost = [Buf("post%d" % i) for i in range(2)]
    fix = A.alloc("pfix", [128, 16], F32)
    Bfix = Buf("pfix")
    ok = 0
    for pc in range(2):
        P.pool(lambda e: e.memset(PW[:], 0.0), reads=[BPW], writes=[BPW])
        for half in range(2):
            gidx = pc * 2 + half
            P.dma("pool", PW[half * 64:(half + 1) * 64, half * 64:(half + 1) * 64], c.pool_w[l, gidx], reads=[BPW], writes=[BPW])
        for g in range(8):
            b = c.bank()
            inproj_fm(c, mctx, 768 + pc * 128, 128, g, b)
            P.act(lambda e, b=b, g=g: e.copy(out=p32[:, g * 512:(g + 1) * 512], in_=c.ps[:, b, :]), reads=[c.Bps[b]], writes=[Bp])

        def shifted_add(dst, Bdst, src, Bsrc, k):
            P.dve(lambda e: e.tensor_tensor(out=dst[:, k:S], in0=src[:, k:S], in1=src[:, 0:S - k], op=ALU.add),
                  reads=[Bsrc], writes=[Bdst])
            P.act(lambda e: e.copy(out=dst[:, 0:k], in_=src[:, 0:k]), reads=[Bsrc, Bdst], writes=[Bdst])
        shifted_add(bA, BA_, p32, Bp, 1)
        shifted_add(bB, BB_, bA, BA_, 2)
        if pc == 0:
            lo, Blo, hi, Bhi = bA, BA_, bB, BB_
        else:
            shifted_add(bA, BA_, bB, BB_, 4)
            shifted_add(bB, BB_, bA, BA_, 8)
            lo, Blo, hi, Bhi = bA, BA_, bB, BB_
        for half, (ws, Bws) in enumerate(((lo, Blo), (hi, Bhi))):
            pr = slice(half * 64, half * 64 + 64)
            P.dve(lambda e, pr=pr, ws=ws: e.scalar_tensor_tensor(out=dT[pr, :], in0=ws[pr, :], scalar=c.invw[pr, pc:pc + 1],
                                                                 in1=p32[pr, :], op0=ALU.mult, op1=ALU.subtract),
                  reads=[Bws, Bp, c.Bpoolc, Bd], writes=[Bd])
            P.dve(lambda e, pr=pr, ws=ws: e.tensor_tensor(out=fix[pr, :], in0=ws[pr, 0:16], in1=c.invcnt[pr, pc, :], op=ALU.mult),
                  reads=[Bws, c.Bpoolc, Bfix], writes=[Bfix])
            P.dve(lambda e, pr=pr: e.tensor_tensor(out=dT[pr, 0:16], in0=fix[pr, :], in1=p32[pr, 0:16], op=ALU.subtract),
                  reads=[Bfix, Bp, Bd], writes=[Bd])
        for g in range(8):
            b = c.bank()
            sl = slice(g * 512, (g + 1) * 512)
            P.pe(lambda e, b=b, sl=sl: e.matmul(c.ps[:, b, :], PW[:], dT[:, sl], start=True, stop=True),
                 reads=[BPW, Bd], writes=[c.Bps[b]])
            o_t, Bo = ost[ok % 2], Bost[ok % 2]
            ok += 1
            P.act(lambda e, b=b, o_t=o_t: e.activation(out=o_t[:], in_=c.ps[:, b, :], func=AF.Identity,
                                                       scale=c.vecs[:, PSC + l * 2 + pc:PSC + l * 2 + pc + 1]),
                  reads=[c.Bps[b], c.Bvecs], writes=[Bo])
            P.dma("sp", c.mixT[(2 + pc) * 128:(3 + pc) * 128, sl], o_t[:], reads=[Bo], writes=[c.BmixT[2 + pc]])
    A.release(m)


def mlstm(c, l, mctx):
    nc, P, A = c.nc, c.P, c.A
    m = A.mark()
    uT, BuT, Win, BWin = mctx.uT, mctx.BuT, mctx.Win, mctx.BWin
    TOK = A.alloc("TOK", [128, 32, 12], F32)
    BTOK = Buf("TOK")
    DEC = A.alloc("DEC", [128, 4, 64], F32)
    BDEC = Buf("DEC")
    Wqkv = A.alloc("Wqkv", [128, 12, 128], BF16)
    BWq = Buf("Wqkv")
    P.dma("pool", Wqkv[:], c.qkv_w[l].rearrange("q h d e -> d (q h) e"), writes=[BWq])
    mg = A.mark()
    t0 = A.alloc("g0", [4, S], F32)
    t1 = A.alloc("g1", [4, S], F32)
    t2 = A.alloc("g2", [4, S], F32)
    t3 = A.alloc("g3", [4, S], F32)
    t4 = A.alloc("g4", [4, S], F32)
    onesr = A.alloc("g1s", [4, 512], F32)
    B0, B1, B2, B3, B4, Bon = Buf("g0"), Buf("g1"), Buf("g2"), Buf("g3"), Buf("g4"), Buf("g1s")
    rst = A.alloc("rst", [4, 64], F32)
    dec = A.alloc("decr", [4, 64], F32)
    nbf = A.alloc("nbf", [4, 1], F32)
    Brst, Bdec, Bnbf = Buf("rst"), Buf("decr"), Buf("nbf")
    gb = c.gateb
    P.dve(lambda e: e.memset(onesr[:], 1.0), writes=[Bon])
    P.dve(lambda e: e.tensor_scalar(out=nbf[:], in0=gb[:, l * 2 + 1:l * 2 + 2], scalar1=-1.0, scalar2=None, op0=ALU.mult),
          reads=[c.Bgateb], writes=[Bnbf])
    for g in range(8):
        sl = slice(g * 512, (g + 1) * 512)
        b = c.bank()
        inproj_fm(c, mctx, 2048, 4, g, b)
        P.act(lambda e, b=b, sl=sl: e.activation(out=t0[:, sl], in_=c.ps[0:4, b, :], func=AF.Identity,
                                                 bias=gb[:, l * 2:l * 2 + 1], scale=1.0),
              reads=[c.Bps[b], c.Bgateb], writes=[B0])
        b = c.bank()
        inproj_fm(c, mctx, 2052, 4, g, b)
        P.act(lambda e, b=b, sl=sl: e.activation(out=t1[:, sl], in_=c.ps[0:4, b, :], func=AF.Exp, bias=nbf[:], scale=-1.0),
              reads=[c.Bps[b], Bnbf], writes=[B1])
    P.act(lambda e: e.activation(out=t1[:], in_=t1[:], func=AF.Ln, bias=1.0), reads=[B1], writes=[B1])
    for g in range(8):
        sl = slice(g * 512, (g + 1) * 512)
        init = 0.0 if g == 0 else t2[:, g * 512 - 1:g * 512]
        P.dve(lambda e, sl=sl, init=init: e.tensor_tensor_scan(out=t2[:, sl], data0=onesr[:], data1=t1[:, sl], initial=init,
                                                               op0=ALU.mult, op1=ALU.add), reads=[Bon, B1, B2], writes=[B2])
    P.dve(lambda e: e.tensor_tensor(out=t0[:], in0=t0[:], in1=t2[:], op=ALU.add), reads=[B0, B2], writes=[B0])
    for g in range(8):
        sl = slice(g * 512, (g + 1) * 512)
        init = 0.0 if g == 0 else t1[:, g * 512 - 1:g * 512]
        P.dve(lambda e, sl=sl, init=init: e.tensor_tensor_scan(out=t1[:, sl], data0=t0[:, sl], data1=t0[:, sl], initial=init,
                                                               op0=ALU.max, op1=ALU.max), reads=[B0, B1], writes=[B1])
    Rv = t1[:, :].rearrange("p (c l) -> p c l", l=64)
    Rend = Rv[:, :, 63]
    P.dve(lambda e: e.memset(rst[:, 0:1], 0.0), writes=[Brst])
    P.dve(lambda e: e.tensor_copy(out=rst[:, 1:64], in_=Rv[:, 0:63, 63]), reads=[B1, Brst], writes=[Brst])
    P.dve(lambda e: e.tensor_tensor(out=dec[:], in0=rst[:], in1=Rend, op=ALU.subtract), reads=[Brst, B1], writes=[Bdec])
    P.act(lambda e: e.activation(out=dec[:], in_=dec[:], func=AF.Exp), reads=[Bdec], writes=[Bdec])
    Gv = t0[:, :].rearrange("p (c l) -> p c l", l=64)
    Fv = t2[:, :].rearrange("p (c l) -> p c l", l=64)
    rst_b = rst[:, :].unsqueeze(2).broadcast_to([4, 64, 64])
    rend_b = Rv[:, :, 63:64].broadcast_to([4, 64, 64])
    t3v = t3[:, :].rearrange("p (c l) -> p c l", l=64)
    t4v = t4[:, :].rearrange("p (c l) -> p c l", l=64)
    P.dve(lambda e: e.tensor_tensor(out=t3v, in0=Gv, in1=rst_b, op=ALU.subtract), reads=[B0, Brst], writes=[B3])
    P.act(lambda e: e.activation(out=t3[:], in_=t3[:], func=AF.Exp), reads=[B3], writes=[B3])
    P.dve(lambda e: e.tensor_tensor(out=t4v, in0=Gv, in1=rend_b, op=ALU.subtract), reads=[B0, B1], writes=[B4])
    P.act(lambda e: e.activation(out=t4[:], in_=t4[:], func=AF.Exp), reads=[B4], writes=[B4])
    P.dve(lambda e: e.tensor_tensor(out=Gv, in0=Fv, in1=rst_b, op=ALU.subtract), reads=[B2, Brst, B0, B3, B4], writes=[B0])
    P.act(lambda e: e.activation(out=t0[:], in_=t0[:], func=AF.Exp), reads=[B0], writes=[B0])
    for qi, (src, Bs) in enumerate(((t3, B3), (t4, B4), (t0, B0))):
        b = c.bank()
        for tt in range(32):
            P.pe(lambda e, b=b, tt=tt, src=src: e.transpose(c.ps[:, b, tt * 4:(tt + 1) * 4], src[:, tt * 128:(tt + 1) * 128],
                                                            c.ident[0:4, 0:4]), reads=[Bs, c.Bident], writes=[c.Bps[b]])
        P.dve(lambda e, b=b, qi=qi: e.tensor_copy(out=TOK[:, :, qi * 4:(qi + 1) * 4],
                                                   in_=c.ps[:, b, 0:128].rearrange("p (t h) -> p t h", h=4)),
              reads=[c.Bps[b], BTOK], writes=[BTOK])
    b = c.bank()
    for h in range(4):
        P.pe(lambda e, b=b, h=h: e.matmul(c.ps[:, b, h * 64:(h + 1) * 64], c.selh[:, h, :], dec[:], start=True, stop=True),
             reads=[c.Bselh, Bdec], writes=[c.Bps[b]])
    P.dve(lambda e, b=b: e.tensor_copy(out=DEC[:], in_=c.ps[:, b, 0:256].rearrange("p (h c) -> p h c", c=64)),
          reads=[c.Bps[b]], writes=[BDEC])
    if "gates" in c.debug and l == 0:
        d = c.dbg("TOK", [128, 32, 12])
        P.dma("sp", d, TOK[:], reads=[BTOK])
        d = c.dbg("DEC", [128, 4, 64])
        P.dma("sp", d, DEC[:], reads=[BDEC])
    P.barrier()
    A.release(mg)
    if c.parts is not None and "mlstm" not in c.parts:
        A.release(m)
        return
    xmb = A.alloc("xmb", [128, S], BF16)
    acc32 = A.alloc("cacc", [128, S], F32)
    xcb = A.alloc("xcb", [128, S], BF16)
    qA = A.alloc("qA", [128, S], BF16)
    qB = A.alloc("qB", [128, S], BF16)
    kTb = A.alloc("kTb", [128, S], BF16)
    ktok = A.alloc("ktok", [128, 32, 128], BF16)
    va = A.alloc("va", [128, 32, 129], BF16)
    vb = A.alloc("vb", [128, 32, 129], BF16)
    Bxm, Bacc, Bxc, BqA, BqB, BkT, Bkt, Bva, Bvb = [Buf(n) for n in "xmb cacc xcb qA qB kTb ktok va vb".split()]
    C32 = [A.alloc("C32", [128, 129], F32) for _ in range(2)]
    BC32 = [Buf("C32_%d" % i) for i in range(2)]
    NCB = 4
    Cb = [A.alloc("Cb", [128, 129], BF16) for _ in range(NCB)]
    BCb = [Buf("Cb%d" % i) for i in range(NCB)]
    sT = [A.alloc("sT", [128, 128], BF16) for _ in range(2)]
    BsT = [Buf("sT%d" % i) for i in range(2)]
    sog = [A.alloc("sog", [128, 128], F32) for _ in range(2)]
    Bsog = [Buf("sog%d" % i) for i in range(2)]
    hs = [A.alloc("hs", [128, 128], F32) for _ in range(2)]
    Bhs = [Buf("hs%d" % i) for i in range(2)]
    junk = A.alloc("junk", [128, 128], F32)
    Bjunk = Buf("junk")
    sm = [A.alloc("sm", [128, 4], F32) for _ in range(2)]
    Bsm = [Buf("sm%d" % i) for i in range(2)]
    ost = [A.alloc("most", [128, 512], BF16) for _ in range(2)]
    Bost = [Buf("most%d" % i) for i in range(2)]
    ones129 = A.alloc("o129", [128, 32, 1], BF16)
    cwv = lambda tap, h: c.vecs[:, CW + (l * 4 + tap) * 4 + h: CW + (l * 4 + tap) * 4 + h + 1]
    cbv = lambda h: c.vecs[:, CB + l * 4 + h: CB + l * 4 + h + 1]
    nwv = lambda h: c.vecs[:, NW + l * 4 + h: NW + l * 4 + h + 1]
    skv = lambda h: c.vecs[:, SK + l * 4 + h: SK + l * 4 + h + 1]
    qsc = 128.0 ** -0.5
    kk = [0]
    for h in range(4):
        for g in range(8):
            b = c.bank()
            inproj_fm(c, mctx, 1024 + h * 128, 128, g, b)
            P.act(lambda e, b=b, g=g: e.copy(out=xmb[:, g * 512:(g + 1) * 512], in_=c.ps[:, b, :]), reads=[c.Bps[b]], writes=[Bxm])
        P.dve(lambda e, h=h: e.tensor_scalar(out=acc32[:], in0=xmb[:], scalar1=cwv(3, h), scalar2=None, op0=ALU.mult),
              reads=[Bxm, c.Bvecs, Bacc], writes=[Bacc])
        for lag in range(1, 4):
            P.dve(lambda e, h=h, lag=lag: e.scalar_tensor_tensor(out=acc32[:, lag:S], in0=xmb[:, 0:S - lag], scalar=cwv(3 - lag, h),
                                                                 in1=acc32[:, lag:S], op0=ALU.mult, op1=ALU.add),
                  reads=[Bxm, c.Bvecs, Bacc], writes=[Bacc])
        P.act(lambda e, h=h: e.activation(out=xcb[:], in_=acc32[:], func=AF.Silu, bias=cbv(h)), reads=[Bacc, c.Bvecs, Bxc], writes=[Bxc])
        P.pool(lambda e: e.memset(qA[:], 0.0), reads=[BqA], writes=[BqA])
        P.pool(lambda e: e.memset(qB[:], 0.0), reads=[BqB], writes=[BqB])
        for g in range(8):
            sl = slice(g * 512, (g + 1) * 512)
            b = c.bank()
            P.pe(lambda e, b=b, sl=sl, h=h: e.matmul(c.ps[:, b, :], Wqkv[:, 0 * 4 + h, :], xcb[:, sl], start=True, stop=True),
                 reads=[BWq, Bxc], writes=[c.Bps[b]])
            pv = c.ps[:, b, :].rearrange("p (c two l) -> p c two l", two=2, l=64)
            qAv = qA[:, sl].rearrange("p (c two l) -> p c two l", two=2, l=64)
            qBv = qB[:, sl].rearrange("p (c two l) -> p c two l", two=2, l=64)
            P.act(lambda e, pv=pv, qAv=qAv: e.activation(out=qAv[:, :, 0, :], in_=pv[:, :, 0, :], func=AF.Identity, scale=qsc),
                  reads=[c.Bps[b]], writes=[BqA])
            P.dve(lambda e, pv=pv, qBv=qBv: e.tensor_scalar(out=qBv[:, :, 1, :], in0=pv[:, :, 1, :], scalar1=qsc, scalar2=None, op0=ALU.mult),
                  reads=[c.Bps[b]], writes=[BqB])
            b = c.bank()
            P.pe(lambda e, b=b, sl=sl, h=h: e.matmul(c.ps[:, b, :], Wqkv[:, 1 * 4 + h, :], xcb[:, sl], start=True, stop=True),
                 reads=[BWq, Bxc], writes=[c.Bps[b]])
            P.act(lambda e, b=b, sl=sl: e.copy(out=kTb[:, sl], in_=c.ps[:, b, :]), reads=[c.Bps[b]], writes=[BkT])
        for q4 in range(8):
            b = c.bank()
            for i4 in range(4):
                tt = q4 * 4 + i4
                P.pe(lambda e, b=b, i4=i4, tt=tt, h=h: e.matmul(c.ps[:, b, i4 * 128:(i4 + 1) * 128], xcb[:, tt * 128:(tt + 1) * 128],
                                                                 Wqkv[:, 1 * 4 + h, :], start=True, stop=True),
                     reads=[BWq, Bxc], writes=[c.Bps[b]])
            P.act(lambda e, b=b, q4=q4: e.copy(out=ktok[:, q4 * 4:(q4 + 1) * 4, :], in_=c.ps[:, b, :].rearrange("p (i f) -> p i f", f=128)),
                  reads=[c.Bps[b]], writes=[Bkt])
            b = c.bank()
            for i4 in range(4):
                tt = q4 * 4 + i4
                P.pe(lambda e, b=b, i4=i4, tt=tt, h=h: e.matmul(c.ps[:, b, i4 * 128:(i4 + 1) * 128], xmb[:, tt * 128:(tt + 1) * 128],
                                                                 Wqkv[:, 2 * 4 + h, :], start=True, stop=True),
                     reads=[BWq, Bxm], writes=[c.Bps[b]])
            psv = c.ps[:, b, :].rearrange("p (i f) -> p i f", f=128)
            wa_b = TOK[:, q4 * 4:(q4 + 1) * 4, 0 + h:0 + h + 1].broadcast_to([128, 4, 128])
            wb_b = TOK[:, q4 * 4:(q4 + 1) * 4, 4 + h:4 + h + 1].broadcast_to([128, 4, 128])
            P.dve(lambda e, psv=psv, q4=q4, wa_b=wa_b: e.tensor_tensor(out=va[:, q4 * 4:(q4 + 1) * 4, 0:128], in0=psv, in1=wa_b, op=ALU.mult),
                  reads=[c.Bps[b], BTOK], writes=[Bva])
            P.dve(lambda e, psv=psv, q4=q4, wb_b=wb_b: e.tensor_tensor(out=vb[:, q4 * 4:(q4 + 1) * 4, 0:128], in0=psv, in1=wb_b, op=ALU.mult),
                  reads=[c.Bps[b], BTOK], writes=[Bvb])
        P.act(lambda e, h=h: e.copy(out=va[:, :, 128:129], in_=TOK[:, :, 0 + h:0 + h + 1]), reads=[BTOK, Bva], writes=[Bva])
        P.act(lambda e, h=h: e.copy(out=vb[:, :, 128:129], in_=TOK[:, :, 4 + h:4 + h + 1]), reads=[BTOK, Bvb], writes=[Bvb])
        P.act(lambda e, h=h: e.activation(out=xcb[:], in_=xcb[:], func=AF.Identity, scale=skv(h)), reads=[Bxc, c.Bvecs], writes=[Bxc])
        P.dve(lambda e: e.memset(C32[0][:], 0.0), reads=[BC32[0]], writes=[BC32[0]])
        cprev = {}
        cistate = [0]

        def stage_a(tt, h=h, cprev=cprev, cistate=cistate):
            sl = slice(tt * 128, (tt + 1) * 128)
            base = (tt % 2) * 4
            b1, b2, bo = base, base + 1, base + 2
            si = tt % 2
            P.pe(lambda e: e.matmul(c.ps[:, b1, 0:128], kTb[:, sl], qA[:, sl], start=True, stop=False),
                 reads=[BkT, BqA], writes=[c.Bps[b1]])
            P.pe(lambda e: e.matmul(c.ps[:, b1, 0:128], kTb[:, sl], qB[:, sl], start=False, stop=True),
                 reads=[BkT, BqB], writes=[c.Bps[b1]])
            for kc in range(8):
                P.pe(lambda e, kc=kc: e.matmul(c.ps[:, b1, 128:256], uT[:, kc, sl], Win[:, kc, 1536 + h * 128:1536 + (h + 1) * 128],
                                               start=(kc == 0), stop=(kc == 7)), reads=BuT + [BWin], writes=[c.Bps[b1]])
            s_t, Bs = sT[si], BsT[si]
            P.dve(lambda e: e.tensor_tensor(out=s_t[:], in0=c.ps[:, b1, 0:128], in1=c.lmask[:], op=ALU.mult),
                  reads=[c.Bps[b1], c.Blmask], writes=[Bs])
            so, Bso = sog[si], Bsog[si]
            P.act(lambda e: e.activation(out=so[:], in_=c.ps[:, b1, 128:256], func=AF.Sigmoid), reads=[c.Bps[b1]], writes=[Bso])
            for half in range(2):
                ch = tt * 2 + half
                pr = slice(half * 64, half * 64 + 64)
                if ch < 63:
                    bdc = b1 if half == 0 else b2
                    P.pe(lambda e, pr=pr, bdc=bdc: e.matmul(c.ps[:, bdc, 256:385], ktok[pr, tt, :], vb[pr, tt, :],
                                                            start=True, stop=True), reads=[Bkt, Bvb], writes=[c.Bps[bdc]])
            for half in range(2):
                ch = tt * 2 + half
                if ch >= 1:
                    ci = cistate[0]
                    cistate[0] += 1
                    cbt, Bcb = Cb[ci % NCB], BCb[ci % NCB]
                    src32, Bsrc = C32[ch % 2], BC32[ch % 2]
                    P.act(lambda e, cbt=cbt, src32=src32: e.copy(out=cbt[:], in_=src32[:]), reads=[Bsrc], writes=[Bcb])
                    cprev[ch] = (cbt, Bcb)
                if ch < 63:
                    cur, Bcur = C32[ch % 2], BC32[ch % 2]
                    nxt, Bnxt = C32[(ch + 1) % 2], BC32[(ch + 1) % 2]
                    bdc = b1 if half == 0 else b2
                    P.dve(lambda e, cur=cur, nxt=nxt, ch=ch, bdc=bdc: e.scalar_tensor_tensor(
                        out=nxt[:], in0=cur[:], scalar=DEC[:, h, ch:ch + 1], in1=c.ps[:, bdc, 256:385],
                        op0=ALU.mult, op1=ALU.add), reads=[Bcur, BDEC, c.Bps[bdc]], writes=[Bnxt])
            mm = [(s_t[:], va[:, tt, :], [Bs, Bva])]
            if tt * 2 in cprev:
                mm.append((qA[:, sl], cprev[tt * 2][0][:], [BqA, cprev[tt * 2][1]]))
            mm.append((qB[:, sl], cprev[tt * 2 + 1][0][:], [BqB, cprev[tt * 2 + 1][1]]))
            for i, (lt, rh, rd) in enumerate(mm):
                P.pe(lambda e, lt=lt, rh=rh, i=i, n=len(mm): e.matmul(c.ps[:, bo, 0:129], lt, rh, start=(i == 0), stop=(i == n - 1)),
                     reads=rd, writes=[c.Bps[bo]])

        def stage_b(tt, h=h):
            sl = slice(tt * 128, (tt + 1) * 128)
            base = (tt % 2) * 4
            bo, bt = base + 2, base + 3
            si = tt % 2
            so, Bso = sog[si], Bsog[si]
            sm_t, Bsm_t = sm[si], Bsm[si]
            h_t, Bh_t = hs[si], Bhs[si]
            P.act(lambda e: e.activation(out=sm_t[:, 0:1], in_=c.ps[:, bo, 128:129], func=AF.Abs),
                  reads=[c.Bps[bo]], writes=[Bsm_t])
            P.dve(lambda e: e.tensor_tensor(out=sm_t[:, 0:1], in0=sm_t[:, 0:1], in1=TOK[:, tt, 8 + h:8 + h + 1], op=ALU.max),
                  reads=[Bsm_t, BTOK], writes=[Bsm_t])
            P.dve(lambda e: e.reciprocal(out=sm_t[:, 1:2], in_=sm_t[:, 0:1]), reads=[Bsm_t], writes=[Bsm_t])
            P.dve(lambda e: e.scalar_tensor_tensor(out=h_t[:], in0=c.ps[:, bo, 0:128], scalar=sm_t[:, 1:2],
                                                   in1=so[:], op0=ALU.mult, op1=ALU.mult),
                  reads=[c.Bps[bo], Bsm_t, Bso], writes=[Bh_t])
            P.dve(lambda e: e.memset(sm_t[:, 2:3], 0.0), reads=[Bsm_t], writes=[Bsm_t])
            P.act(lambda e: e.activation(out=junk[:], in_=h_t[:], func=AF.Square, accum_out=sm_t[:, 2:3]),
                  reads=[Bh_t, Bjunk], writes=[Bjunk, Bsm_t])
            P.act(lambda e: e.activation(out=sm_t[:, 3:4], in_=sm_t[:, 2:3], func=AF.Sqrt, scale=1.0 / 128, bias=EPS),
                  reads=[Bsm_t], writes=[Bsm_t])
            P.dve(lambda e: e.reciprocal(out=sm_t[:, 3:4], in_=sm_t[:, 3:4]), reads=[Bsm_t], writes=[Bsm_t])
            P.dve(lambda e: e.tensor_scalar(out=h_t[:], in0=h_t[:], scalar1=sm_t[:, 3:4], scalar2=None, op0=ALU.mult),
                  reads=[Bsm_t, Bh_t], writes=[Bh_t])
            P.pe(lambda e: e.transpose(c.ps[:, bt, 0:128], h_t[:], c.ident[:]), reads=[Bh_t, c.Bident], writes=[c.Bps[bt]])
            g = tt // 4
            o_t, Bo = ost[g % 2], Bost[g % 2]
            P.dve(lambda e: e.scalar_tensor_tensor(
                out=o_t[:, (tt % 4) * 128:(tt % 4 + 1) * 128], in0=c.ps[:, bt, 0:128], scalar=nwv(h), in1=xcb[:, sl],
                op0=ALU.mult, op1=ALU.add), reads=[c.Bps[bt], c.Bvecs, Bxc, Bo], writes=[Bo])
            if tt % 4 == 3:
                P.dma("sp", c.mixT[(4 + h) * 128:(5 + h) * 128, g * 512:(g + 1) * 512], o_t[:], reads=[Bo], writes=[c.BmixT[4 + h]])

        stage_a(0)
        for tt in range(32):
            if tt + 1 < 32:
                stage_a(tt + 1)
            stage_b(tt)
    A.release(m)


def outproj(c, l):
    nc, P, A = c.nc, c.P, c.A
    m = A.mark()
    TT = 512
    Wo = A.alloc("Wo", [128, 8, D], BF16)
    BWo = Buf("Wo")
    P.dma("pool", Wo[:], c.mix_out[l].rearrange("(kc p) n -> p kc n", p=128), writes=[BWo])
    nb = NormBufs(c, TT, "o", ntmp=2)
    ht = [A.alloc("oht", [128, 8, TT], F32) for _ in range(2)]
    Bht = [Buf("oht%d" % i) for i in range(2)]
    mx = [A.alloc("omx", [128, 8, TT], BF16) for _ in range(2)]
    Bmx = [Buf("omx%d" % i) for i in range(2)]
    y = [A.alloc("oy", [128, 8, TT], F32) for _ in range(2)]
    By = [Buf("oy%d" % i) for i in range(2)]
    NG = S // TT

    def load(g):
        sl = slice(g * TT, (g + 1) * TT)
        P.dma("sp", ht[g % 2][:], hT_tile(c, g * TT, TT), reads=hT_bufs(c, g * TT, TT), writes=[Bht[g % 2]])
        P.dma("sp", mx[g % 2][:], c.mixT.rearrange("(kc p) t -> p kc t", p=128)[:, :, sl], reads=c.BmixT, writes=[Bmx[g % 2]])

    def mm(g):
        mxi, Bm = mx[g % 2], Bmx[g % 2]
        for dc in range(8):
            b = c.bank()
            for kc in range(8):
                P.pe(lambda e, b=b, dc=dc, kc=kc, mxi=mxi: e.matmul(c.ps[:, b, :], Wo[:, kc, dc * 128:(dc + 1) * 128], mxi[:, kc, :],
                                                                   start=(kc == 0), stop=(kc == 7)), reads=[BWo, Bm], writes=[c.Bps[b]])
            P.act(lambda e, b=b, dc=dc: e.copy(out=y[g % 2][:, dc, :], in_=c.ps[:, b, :]), reads=[c.Bps[b]], writes=[By[g % 2]])

    def post(g):
        post_stage(c, nb, y[g % 2][:], By[g % 2], ht[g % 2][:], Bht[g % 2], l, 1)
        P.dma("sp", hT_tile(c, g * TT, TT), ht[g % 2][:], reads=[Bht[g % 2]], writes=hT_bufs(c, g * TT, TT))

    load(0)
    mm(0)
    for g in range(NG):
        if g + 1 < NG:
            load(g + 1)
            mm(g + 1)
        post(g)
    A.release(m)


def pack_inputs(inputs, b):
    f = np.float32
    vecs = np.zeros((128, NV), f)
    vecs[:, CT:CT + 8] = inputs["c"][b].reshape(8, 128).T
    for l in range(NL):
        vecs[:, ADAB + l * 72:ADAB + (l + 1) * 72] = inputs["ada_b"][l].reshape(72, 128).T
        for j in range(3):
            o = (l * 3 + j) * 8
            vecs[:, PREW + o:PREW + o + 8] = inputs["pre_norm_w"][l, j].reshape(8, 128).T
            vecs[:, POSTW + o:POSTW + o + 8] = inputs["post_norm_w"][l, j].reshape(8, 128).T
        vecs[:, PSC + l * 2:PSC + l * 2 + 2] = inputs["pool_scale"][l].reshape(2, 128).T
        for tap in range(4):
            vecs[:, CW + (l * 4 + tap) * 4:CW + (l * 4 + tap) * 4 + 4] = inputs["mlstm_conv_w"][l, tap].reshape(4, 128).T
        vecs[:, CB + l * 4:CB + l * 4 + 4] = inputs["mlstm_conv_b"][l].reshape(4, 128).T
        vecs[:, NW + l * 4:NW + l * 4 + 4] = inputs["mlstm_norm_w"][l].reshape(4, 128).T
        vecs[:, SK + l * 4:SK + l * 4 + 4] = inputs["mlstm_skip"][l].reshape(4, 128).T
    gateb = np.ascontiguousarray(inputs["mlstm_gate_b"].transpose(2, 0, 1).reshape(4, 8)).astype(f)
    return vecs, gateb


_CACHE = {}


def kernel(**inputs):
    inputs = {k: np.asarray(v) for k, v in inputs.items()}
    if "nc" not in _CACHE:
        _CACHE["nc"] = build()[0]
    nc = _CACHE["nc"]
    shared = {
        "ada_w": np.ascontiguousarray(inputs["ada_w"], dtype=np.float32),
        "ffn_up": np.ascontiguousarray(inputs["ffn_up"], dtype=np.float32),
        "ffn_down": np.ascontiguousarray(inputs["ffn_down"], dtype=np.float32),
        "mix_in_w": np.ascontiguousarray(inputs["mix_in_w"], dtype=np.float32),
        "mix_out_w": np.ascontiguousarray(inputs["mix_out_w"], dtype=np.float32),
        "pool_w": np.ascontiguousarray(inputs["pool_w"], dtype=np.float32),
        "qkv_w": np.ascontiguousarray(inputs["mlstm_qkv_w"], dtype=np.float32),
    }
    in_maps = []
    for b in range(8):
        vecs, gateb = pack_inputs(inputs, b)
        d = dict(shared)
        d["x"] = np.ascontiguousarray(inputs["x"][b], dtype=np.float32)
        d["vecs"] = vecs
        d["gateb"] = gateb
        in_maps.append(d)
    res = run_bass_kernel_spmd(nc, in_maps, core_ids=list(range(8)))
    return np.stack([np.asarray(r["out"]) for r in res.results], axis=0).astype(np.float32)
```
